# Optimizing a Trainium2 kernel written in Bass

```python
import math
import jax, jax.numpy as jnp
from jax import lax
import numpy as np

D_MODEL = 1024
BATCH = 16
SEQ = 2048
DEPTH = 1

MLA_HEADS = 8
MLA_Q_RANK = 256
MLA_KV_RANK = 128
MLA_NOPE = 64
MLA_ROPE = 32
MLA_V = 64
MLA_SCALE = 1.0 / math.sqrt(MLA_NOPE + MLA_ROPE)
DIFF_HEADS = 4
DIFF_QK = 64
DIFF_V = 2 * DIFF_QK
DIFF_SCALE = 1.0 / math.sqrt(DIFF_QK)
MIX_WIDTH = MLA_HEADS * MLA_V + DIFF_HEADS * DIFF_V
DIFF_QK_COLS = DIFF_HEADS * 2 * DIFF_QK
DIFF_V_COLS = DIFF_HEADS * DIFF_V
IN_COLS = MLA_Q_RANK + MLA_KV_RANK + MLA_ROPE + 2 * DIFF_QK_COLS + DIFF_V_COLS
IN_SPLITS = tuple(int(s) for s in np.cumsum([MLA_Q_RANK, MLA_KV_RANK, MLA_ROPE, DIFF_QK_COLS, DIFF_QK_COLS]))
N_BUCKETS = 32
MAX_DISTANCE = 128
N_EXPERTS = 16
EXPERT_FF = 2048
CAPACITY_FACTOR = 2
Q_BLOCK = 128
ROPE_THETA = 10000.0
LN_EPS = 1e-5
RMS_EPS = 1e-6
DEEPNORM_ALPHA = (2.0 * DEPTH) ** 0.25
DEEPNORM_BETA = (8.0 * DEPTH) ** -0.25

kernel_name = "hybrid_mla_diffattn_ec_moe_encoder"


def layer_norm(x, g, b):
    xf = x.astype(jnp.float32)
    mu = jnp.mean(xf, axis=-1, keepdims=True)
    var = jnp.mean(jnp.square(xf - mu), axis=-1, keepdims=True)
    y = (xf - mu) * lax.rsqrt(var + LN_EPS)
    return (y * g.astype(jnp.float32) + b.astype(jnp.float32)).astype(x.dtype)


def rms_norm(x, g):
    xf = x.astype(jnp.float32)
    y = xf * lax.rsqrt(jnp.mean(jnp.square(xf), axis=-1, keepdims=True) + RMS_EPS)
    return (y * g.astype(jnp.float32)).astype(x.dtype)


def rope(t, pos):
    half = t.shape[-1] // 2
    freqs = ROPE_THETA ** (-jnp.arange(half, dtype=jnp.float32) / half)
    ang = pos.astype(jnp.float32)[..., None] * freqs
    cos = jnp.cos(ang)[:, :, None, :].astype(t.dtype)
    sin = jnp.sin(ang)[:, :, None, :].astype(t.dtype)
    t1, t2 = t[..., :half], t[..., half:]
    return jnp.concatenate([t1 * cos - t2 * sin, t1 * sin + t2 * cos], axis=-1)


def t5_bucket(rel):
    nb = N_BUCKETS // 2
    max_exact = nb // 2
    ret = jnp.where(rel > 0, nb, 0)
    n = jnp.abs(rel)
    nf = jnp.maximum(n, 1).astype(jnp.float32)
    large = max_exact + (jnp.log(nf / max_exact) / math.log(MAX_DISTANCE / max_exact)
                         * (nb - max_exact)).astype(jnp.int32)
    large = jnp.minimum(large, nb - 1)
    return ret + jnp.where(n < max_exact, n, large)


def attention_mixer(u, positions, rel_bias, w_in, mla_q_norm, w_uq, mla_kv_norm, w_ukv,
                    diff_lq1, diff_lk1, diff_lq2, diff_lk2, diff_subln, w_out, layer_idx):
    B, S, _ = u.shape
    nblk = S // Q_BLOCK
    proj = jnp.einsum('bsd,dn->bsn', u, w_in)
    cq, ckv, k_rope, dq, dk, dv = jnp.split(proj, IN_SPLITS, axis=-1)

    q = jnp.einsum('bsr,rn->bsn', rms_norm(cq, mla_q_norm), w_uq).reshape(B, S, MLA_HEADS, MLA_NOPE + MLA_ROPE)
    q_nope = q[..., :MLA_NOPE]
    q_rope = rope(q[..., MLA_NOPE:], positions)
    kv = jnp.einsum('bsr,rn->bsn', rms_norm(ckv, mla_kv_norm), w_ukv).reshape(B, S, MLA_HEADS, MLA_NOPE + MLA_V)
    k_nope, v_mla = kv[..., :MLA_NOPE], kv[..., MLA_NOPE:]
    k_rope = rope(k_rope[:, :, None, :], positions)[:, :, 0, :]

    dq = dq.reshape(B, S, DIFF_HEADS, 2, DIFF_QK)
    dk = dk.reshape(B, S, DIFF_HEADS, 2, DIFF_QK)
    dv = dv.reshape(B, S, DIFF_HEADS, DIFF_V)
    lambda_init = 0.8 - 0.6 * math.exp(-0.3 * layer_idx)
    lam = (jnp.exp(jnp.sum(diff_lq1.astype(jnp.float32) * diff_lk1.astype(jnp.float32)))
           - jnp.exp(jnp.sum(diff_lq2.astype(jnp.float32) * diff_lk2.astype(jnp.float32)))
           + lambda_init)

    def to_blocks(t):
        return jnp.moveaxis(t.reshape((B, nblk, Q_BLOCK) + t.shape[2:]), 1, 0)

    def one_block(args):
        qn, qr, dqb, qp = args
        s = (jnp.einsum('bqhd,bkhd->bhqk', qn, k_nope)
             + jnp.einsum('bqhr,bkr->bhqk', qr, k_rope)) * MLA_SCALE
        p = jax.nn.softmax(s.astype(jnp.float32), axis=-1).astype(v_mla.dtype)
        o_mla = jnp.einsum('bhqk,bkhd->bqhd', p, v_mla).reshape(B, Q_BLOCK, MLA_HEADS * MLA_V)
        rel = positions[:, None, :] - qp[:, :, None]
        bias = jnp.transpose(rel_bias[t5_bucket(rel)], (0, 3, 1, 2))
        s12 = (jnp.einsum('bqhmd,bkhmd->bmhqk', dqb, dk) * DIFF_SCALE
               + bias[:, None].astype(dqb.dtype))
        p12 = jax.nn.softmax(s12.astype(jnp.float32), axis=-1)
        a = (p12[:, 0] - lam * p12[:, 1]).astype(dv.dtype)
        o_d = jnp.einsum('bhqk,bkhd->bqhd', a, dv)
        o_d = rms_norm(o_d, diff_subln) * (1.0 - lambda_init)
        return jnp.concatenate([o_mla, o_d.reshape(B, Q_BLOCK, DIFF_HEADS * DIFF_V)], axis=-1)

    out = lax.map(one_block, (to_blocks(q_nope), to_blocks(q_rope), to_blocks(dq), to_blocks(positions)))
    out = jnp.moveaxis(out, 0, 1).reshape(B, S, MIX_WIDTH)
    return jnp.einsum('bsm,md->bsd', out, w_out)


def expert_choice_ffn(u, w_router, w_gate, w_up, w_down):
    B, S, D = u.shape
    cap = CAPACITY_FACTOR * S // N_EXPERTS
    logits = jnp.einsum('bsd,de->bse', u, w_router).astype(jnp.float32)
    aff = jax.nn.softmax(logits, axis=-1)
    g, idx = lax.top_k(jnp.swapaxes(aff, 1, 2), cap)
    xs = jax.vmap(lambda ub, ib: ub[ib])(u, idx)
    h = jax.nn.silu(jnp.einsum('becd,edf->becf', xs, w_gate)) * jnp.einsum('becd,edf->becf', xs, w_up)
    y = jnp.einsum('becf,efd->becd', h, w_down) * g[..., None].astype(u.dtype)
    flat_idx = (jnp.arange(B, dtype=jnp.int32)[:, None, None] * S + idx).reshape(-1)
    out = jnp.zeros((B * S, D), y.dtype).at[flat_idx].add(y.reshape(-1, D))
    return out.reshape(B, S, D)


def setup_inputs(seed: int = 0) -> dict:
    key = jax.random.key(seed)
    ks = jax.random.split(key, 32)
    f32 = jnp.float32
    nrm = lambda k, shape, s: jax.random.normal(k, shape, f32) * s
    L, D = DEPTH, D_MODEL
    pos_off = jax.random.randint(ks[2], (BATCH, 1), 0, 4096, dtype=jnp.int32)
    return {
        "x": nrm(ks[0], (BATCH, SEQ, D), 1.0),
        "c": nrm(ks[1], (BATCH, D), 1.0),
        "positions": pos_off + jnp.arange(SEQ, dtype=jnp.int32)[None, :],
        "rel_bias": nrm(ks[3], (N_BUCKETS, DIFF_HEADS), 0.5),
        "w_ada": nrm(ks[4], (L, D, 6 * D), 0.5 * D ** -0.5),
        "b_ada": nrm(ks[5], (L, 6 * D), 0.02),
        "w_in": nrm(ks[6], (L, D, IN_COLS), D ** -0.5),
        "mla_q_norm": 1.0 + nrm(ks[7], (L, MLA_Q_RANK), 0.02),
        "w_uq": nrm(ks[8], (L, MLA_Q_RANK, MLA_HEADS * (MLA_NOPE + MLA_ROPE)), MLA_Q_RANK ** -0.5),
        "mla_kv_norm": 1.0 + nrm(ks[9], (L, MLA_KV_RANK), 0.02),
        "w_ukv": nrm(ks[10], (L, MLA_KV_RANK, MLA_HEADS * (MLA_NOPE + MLA_V)), MLA_KV_RANK ** -0.5),
        "diff_lq1": nrm(ks[11], (L, DIFF_QK), 0.1),
        "diff_lk1": nrm(ks[12], (L, DIFF_QK), 0.1),
        "diff_lq2": nrm(ks[13], (L, DIFF_QK), 0.1),
        "diff_lk2": nrm(ks[14], (L, DIFF_QK), 0.1),
        "diff_subln": 1.0 + nrm(ks[15], (L, DIFF_V), 0.02),
        "w_out": nrm(ks[16], (L, MIX_WIDTH, D), DEEPNORM_BETA * MIX_WIDTH ** -0.5),
        "ln1_g": 1.0 + nrm(ks[17], (L, D), 0.02),
        "ln1_b": nrm(ks[18], (L, D), 0.02),
        "w_router": nrm(ks[19], (L, D, N_EXPERTS), D ** -0.5),
        "w_gate": nrm(ks[20], (L, N_EXPERTS, D, EXPERT_FF), D ** -0.5),
        "w_up": nrm(ks[21], (L, N_EXPERTS, D, EXPERT_FF), D ** -0.5),
        "w_down": nrm(ks[22], (L, N_EXPERTS, EXPERT_FF, D), DEEPNORM_BETA * EXPERT_FF ** -0.5),
        "ln2_g": 1.0 + nrm(ks[23], (L, D), 0.02),
        "ln2_b": nrm(ks[24], (L, D), 0.02),
    }


def reference(x, c, positions, rel_bias, w_ada, b_ada, w_in, mla_q_norm, w_uq, mla_kv_norm, w_ukv,
              diff_lq1, diff_lk1, diff_lq2, diff_lk2, diff_subln, w_out, ln1_g, ln1_b,
              w_router, w_gate, w_up, w_down, ln2_g, ln2_b):
    c_act = jax.nn.silu(c)
    for l in range(DEPTH):
        mod = jnp.einsum('bd,dn->bn', c_act, w_ada[l]) + b_ada[l]
        sh_a, sc_a, g_a, sh_f, sc_f, g_f = jnp.split(mod[:, None, :], 6, axis=-1)
        u = x * (1.0 + sc_a) + sh_a
        mix = attention_mixer(u, positions, rel_bias, w_in[l], mla_q_norm[l], w_uq[l], mla_kv_norm[l], w_ukv[l],
                              diff_lq1[l], diff_lk1[l], diff_lq2[l], diff_lk2[l], diff_subln[l], w_out[l], l)
        x = layer_norm(DEEPNORM_ALPHA * x + g_a * mix, ln1_g[l], ln1_b[l])
        u = x * (1.0 + sc_f) + sh_f
        ffn = expert_choice_ffn(u, w_router[l], w_gate[l], w_up[l], w_down[l])
        x = layer_norm(DEEPNORM_ALPHA * x + g_f * ffn, ln2_g[l], ln2_b[l])
    return x
```

```python
import math
import os
from contextlib import ExitStack, contextmanager

import numpy as np
import concourse.bass as bass
import concourse.mybir as mybir
from concourse.bass_utils import run_bass_kernel_spmd

F32 = mybir.dt.float32
BF16 = mybir.dt.bfloat16
I32 = mybir.dt.int32
AF = mybir.ActivationFunctionType
ALU = mybir.AluOpType
AX = mybir.AxisListType

S = 2048
D = 1024
NT = 16
NB = 2
NE = 16
FF = 2048
CAP = 256
ALPHA = 2.0 ** 0.25
MLA_SCALE = 1.0 / math.sqrt(96.0)
DIFF_SCALE = 0.125
LAM_INIT = 0.8 - 0.6 * math.exp(0.0)
LN_EPS = 1e-5
RMS_EPS = 1e-6
TWO_PI_HI = 6.28125
TWO_PI_LO = 2.0 * math.pi - 6.28125
ENG = ("pe", "act", "dve", "pool", "sp")
STRICT_SAME_ENGINE = True
WARM_DUMMY = 0


class Dummy:
    def __getattr__(self, k):
        return self

    def __getitem__(self, k):
        return self

    def __call__(self, *a, **k):
        return self


class Prog:
    def __init__(self, nc, sig=None):
        self.nc = nc
        self.dry = nc is None
        self.sig = sig if sig is not None else set()
        self.need = set()
        self.nops = {e: 0 for e in ENG}
        self.sigcount = {e: 0 for e in ENG}
        self.sigval = {}
        self.res = {}
        self.waited_eng = {e: {} for e in ENG}
        self.waited_dma = {e: {} for e in ENG}
        self.dcount = {}
        self.dsem = {}
        self.stack = [ExitStack()]
        self.esem = {}
        self.ninst = 0
        self._last_compute = {}
        if not self.dry:
            self.engobj = {"pe": nc.tensor, "act": nc.scalar, "dve": nc.vector,
                           "pool": nc.gpsimd, "sp": nc.sync}
            for e in ("pe", "act", "dve", "pool"):
                self.esem[e] = self.stack[0].enter_context(nc.semaphore("sem_" + e))

    def sbuf(self, name, shape, dtype):
        if self.dry:
            return Dummy()
        self.nalloc = getattr(self, "nalloc", 0) + 1
        return self.stack[-1].enter_context(self.nc.sbuf_tensor("%s_%d" % (name, self.nalloc), list(shape), dtype))

    def psum(self, name, shape, dtype):
        if self.dry:
            return Dummy()
        return self.stack[-1].enter_context(self.nc.psum_tensor(name, list(shape), dtype))

    def dram(self, name, shape, dtype, kind="Internal"):
        if self.dry:
            return Dummy()
        return self.nc.dram_tensor(name, list(shape), dtype, kind=kind).ap()

    @contextmanager
    def scope(self):
        self.stack.append(ExitStack())
        try:
            yield
        finally:
            self.barrier()
            self.stack.pop().close()

    def _wait(self, eng, ref):
        if ref[0] == "eng":
            _, pe, idx = ref
            w = self.waited_eng[eng]
            if w.get(pe, -1) >= idx:
                return
            w[pe] = idx
            if self.dry:
                self.need.add((pe, idx))
            else:
                self.engobj[eng].wait_ge(self.esem[pe], self.sigval[(pe, idx)])
        else:
            _, key, _cnt = ref
            cnt = self.dcount[key]
            w = self.waited_dma[eng]
            if w.get(key, 0) >= cnt:
                return
            w[key] = cnt
            if not self.dry:
                self.engobj[eng].wait_ge(self.dsem[key], 16 * cnt)

    def op(self, eng, fn, reads=(), writes=(), dma=None, after=()):
        deps = []
        for ref in after:
            self._wait(eng, ref)
        for k in reads:
            r = self.res.get(k)
            if r is not None and r[0] is not None:
                deps.append((r[0], True))
        for k in writes:
            r = self.res.get(k)
            if r is not None:
                if r[0] is not None:
                    deps.append((r[0], False))
                for rr in r[1].values():
                    deps.append((rr, False))
        for ref, raw in deps:
            if ref[0] == "eng" and dma is None and ref[1] == eng:
                if eng == "pe" or (not raw and not STRICT_SAME_ENGINE):
                    continue
            self._wait(eng, ref)
        idx = self.nops[eng]
        self.nops[eng] += 1
        if dma is not None:
            self.dcount[dma] = self.dcount.get(dma, 0) + 1
            me = ("dma", dma, self.dcount[dma])
            if not self.dry:
                if dma not in self.dsem:
                    self.dsem[dma] = self.stack[0].enter_context(
                        self.nc.semaphore("dsem_%d" % len(self.dsem)))
                ins = fn(self.engobj[eng])
                ins.then_inc(self.dsem[dma], 16)
                self.ninst += 1
        else:
            me = ("eng", eng, idx)
            self._last_compute[eng] = idx
            if not self.dry:
                ins = fn(self.engobj[eng])
                self.ninst += 1
                if (eng, idx) in self.sig:
                    self.sigcount[eng] += 1
                    ins.then_inc(self.esem[eng], 1)
                    self.sigval[(eng, idx)] = self.sigcount[eng]
        for k in writes:
            self.res[k] = [me, {}]
        for k in reads:
            r = self.res.get(k)
            if r is None:
                r = [None, {}]
                self.res[k] = r
            r[1][(me[0], me[1])] = me
        return me

    def barrier(self):
        for f in ENG:
            for e in ("pe", "act", "dve", "pool"):
                li = self._last_compute.get(e)
                if li is None or e == f:
                    continue
                self._wait(f, ("eng", e, li))
            for key in list(self.dcount.keys()):
                self._wait(f, ("dma", key, self.dcount[key]))

    def finish(self):
        for e in ("pe", "act", "dve", "pool"):
            li = self._last_compute.get(e)
            if li is not None:
                self._wait("sp", ("eng", e, li))
        for key in list(self.dcount.keys()):
            self._wait("sp", ("dma", key, self.dcount[key]))


def _t5_bucket_table_np(lo, hi):
    rel = np.arange(lo, hi + 1, dtype=np.int32)
    ret = np.where(rel > 0, 16, 0)
    n = np.abs(rel)
    nf = np.maximum(n, 1).astype(np.float32)
    a = (nf / np.float32(8)).astype(np.float32)
    lg = np.log(a).astype(np.float32)
    r = (lg / np.float32(math.log(128 / 8))).astype(np.float32)
    v = (r * np.float32(8)).astype(np.float32)
    large = np.minimum(8 + v.astype(np.int32), 15)
    return np.asarray(ret + np.where(n < 8, n, large))


def _t5_bucket_table(lo, hi):
    try:
        import jax
        import jax.numpy as jnp
        nb = 16
        max_exact = 8
        with jax.default_device(jax.devices("cpu")[0]):
            rel = jnp.arange(lo, hi + 1, dtype=jnp.int32)
            ret = jnp.where(rel > 0, nb, 0)
            n = jnp.abs(rel)
            nf = jnp.maximum(n, 1).astype(jnp.float32)
            large = max_exact + (jnp.log(nf / max_exact) / math.log(128 / max_exact)
                                 * (nb - max_exact)).astype(jnp.int32)
            large = jnp.minimum(large, nb - 1)
            out = ret + jnp.where(n < max_exact, n, large)
            return np.asarray(out)
    except Exception:
        return _t5_bucket_table_np(lo, hi)


_BUCKETS = None


def _band_steps():
    global _BUCKETS
    if _BUCKETS is None:
        _BUCKETS = _t5_bucket_table(-255, 255)
    tb = _BUCKETS
    steps = []
    for i in range(1, len(tb)):
        if tb[i] != tb[i - 1]:
            rel = -255 + i
            steps.append((rel - 0.5, int(tb[i - 1]), int(tb[i])))
    assert tb[0] == 15 and tb[-1] == 31
    assert tb[255 - 128] == 15 and tb[255 + 128] == 31
    return steps


def build(P, stage="full", dbg=False):
    STG = ["setup", "proj", "attn", "epi", "route", "moe", "full"]
    lvl = STG.index(stage)

    EI = "ExternalInput"
    x_d = P.dram("x", [NB, S, D], F32, EI)
    cT_d = P.dram("cT", [128, 16], F32, EI)
    pos_d = P.dram("pos", [NB, S], I32, EI)
    relb_d = P.dram("rel_bias", [1, 128], F32, EI)
    wada_d = P.dram("w_ada", [D, 6 * D], F32, EI)
    bada_d = P.dram("b_ada", [1, 6 * D], F32, EI)
    win_d = P.dram("w_in_ext", [D, 2048], F32, EI)
    wuq_d = P.dram("w_uq_ext", [256, 1536], F32, EI)
    wukv_d = P.dram("w_ukv_r", [128, 1024], F32, EI)
    qn_d = P.dram("qn_col", [128, 2], F32, EI)
    kvn_d = P.dram("kvn_col", [128, 1], F32, EI)
    lam_d = P.dram("lam_in", [1, 256], F32, EI)
    subln_d = P.dram("subln_col", [128, 1], F32, EI)
    wout_d = P.dram("w_out", [D, D], F32, EI)
    ln1_d = P.dram("ln1", [2, D], F32, EI)
    wr_d = P.dram("w_router", [D, NE], F32, EI)
    wg_d = P.dram("w_gate", [NE, D, FF], F32, EI)
    wu_d = P.dram("w_up", [NE, D, FF], F32, EI)
    wd_d = P.dram("w_down", [NE, FF, D], F32, EI)
    ln2_d = P.dram("ln2", [2, D], F32, EI)
    ident_d = P.dram("ident", [128, 128], F32, EI)
    reliota_d = P.dram("rel_iota", [128, 384], F32, EI)
    cols_d = P.dram("cols", [128, 4], F32, EI)
    slotiota_d = P.dram("slot_iota", [128, 256], F32, EI)
    tokiota_d = P.dram("tok_iota", [128, 32], F32, EI)
    out_d = P.dram("out", [NB, S, D], F32, "ExternalOutput")
    mod_d = P.dram("mod_scr", [NB, 6 * D], F32)
    uf_d = P.dram("uf_scr", [NB * S, D], BF16)
    acc_d = P.dram("acc_scr", [NB * S, D], F32)
    rs_d = P.dram("rs_scr", [8, 512], F32)
    rs2_d = P.dram("rs2_scr", [8, 512], F32)
    dbg_d = {}

    def dbg_out(name, shape, dtype=F32):
        if name not in dbg_d:
            dbg_d[name] = P.dram("dbg_" + name, shape, dtype, "ExternalOutput")
        return dbg_d[name]

    ident_f = P.sbuf("ident_f", [128, 128], F32)
    ident_b = P.sbuf("ident_b", [128, 128], BF16)
    ones_f = P.sbuf("ones_f", [128, 128], F32)
    ones_b = P.sbuf("ones_b", [128, 128], BF16)
    wuq_b = P.sbuf("wuq_b", [128, 2, 1536], BF16)
    wukv_b = P.sbuf("wukv_b", [128, 1024], BF16)
    band = P.sbuf("band", [128, 4, 1152], F32)
    rbb = P.sbuf("rbb", [128, 128], F32)
    rbs = P.sbuf("rbs", [128, 4], F32)
    lamc = P.sbuf("lamc", [128, 8], F32)
    sublnc = P.sbuf("sublnc", [128, 1], F32)
    colc = P.sbuf("colc", [128, 4], F32)
    modcol = P.sbuf("modcol", [128, NB, 16], F32)
    ag = P.sbuf("ag", [128, D], F32)
    ab = P.sbuf("ab", [128, D], F32)
    gfb = P.sbuf("gfb", [128, NB, D], F32)
    slot_iota = P.sbuf("slot_iota_sb", [128, 256], F32)
    tok_iota = P.sbuf("tok_iota_sb", [128, 32], F32)
    idx_i = P.sbuf("idx_i", [128, 64], I32)
    g_sb = P.sbuf("g_sb", [128, 64], F32)
    aff_all = P.sbuf("aff_all", [128, 16, 48], F32)
    wr_sb = P.sbuf("wr_sb", [128, 8, NE], F32)
    ps = [P.psum("ps%d" % i, [128, 512], F32) for i in range(8)]

    class PSPool:
        def __init__(self):
            self.cls = {"s": [0, 1, 2], "a": [3, 4, 5], "m": [6, 7]}
            self.ctr = {k: 0 for k in self.cls}

        def get(self, c):
            lst = self.cls[c]
            i = lst[self.ctr[c] % len(lst)]
            self.ctr[c] += 1
            return i

    PSP = PSPool()

    def dma(eng, out, in_, reads, writes, key):
        P.op(eng, lambda e: e.dma_start(out=out, in_=in_), reads, writes, dma=key)

    def mm(out, lhsT, rhs, start, stop, reads, writes):
        P.op("pe", lambda e: e.matmul(out, lhsT=lhsT, rhs=rhs, start=start, stop=stop), reads, writes)

    def tr(out, in_, ident, reads, writes):
        P.op("pe", lambda e: e.transpose(out=out, in_=in_, identity=ident), reads, writes)

    def act(out, in_, func, reads, writes, bias=None, scale=None, accum_out=None):
        kw = {}
        if bias is not None:
            kw["bias"] = bias
        if scale is not None:
            kw["scale"] = scale
        if accum_out is not None:
            kw["accum_out"] = accum_out
        P.op("act", lambda e: e.activation(out=out, in_=in_, func=func, **kw), reads, writes)

    def ts(eng, out, in0, s1, s2, op0, op1, reads, writes):
        if op1 is None:
            P.op(eng, lambda e: e.tensor_scalar(out=out, in0=in0, scalar1=s1, scalar2=None, op0=op0),
                 reads, writes)
        else:
            P.op(eng, lambda e: e.tensor_scalar(out=out, in0=in0, scalar1=s1, scalar2=s2, op0=op0, op1=op1),
                 reads, writes)

    def tt(eng, out, in0, in1, op, reads, writes):
        P.op(eng, lambda e: e.tensor_tensor(out=out, in0=in0, in1=in1, op=op), reads, writes)

    def stt(out, in0, scalar, in1, op0, op1, reads, writes):
        P.op("dve", lambda e: e.scalar_tensor_tensor(out=out, in0=in0, scalar=scalar, in1=in1, op0=op0, op1=op1),
             reads, writes)

    def cp(eng, out, in_, reads, writes):
        if eng == "act":
            P.op("act", lambda e: e.activation(out=out, in_=in_, func=AF.Copy), reads, writes)
        else:
            P.op(eng, lambda e: e.tensor_copy(out=out, in_=in_), reads, writes)

    def act_pow(out, in_, reads, writes, power, scale=None, bias=None):
        kw = {}
        if scale is not None:
            kw["scale"] = scale
        if bias is not None:
            kw["bias"] = bias
        P.op("act", lambda e: e.activation(out=out, in_=in_, func=AF.Ln, **kw), reads, writes)
        P.op("act", lambda e: e.activation(out=out, in_=out, func=AF.Exp, scale=float(power)), writes, writes)

    def recip(out, in_, reads, writes):
        P.op("dve", lambda e: e.reciprocal(out=out, in_=in_), reads, writes)

    def memset(eng, ap, val, writes):
        P.op(eng, lambda e: e.memset(ap, val), [], writes)

    def dump(name, src_ap, shape, reads, dtype=F32):
        if not dbg:
            return
        d = dbg_out(name, shape, dtype)
        dma("sp", d, src_ap, reads, [], "dbg")

    dma("sp", ident_f[:], ident_d[:, :], [], ["ident_f"], "c0")
    dma("sp", colc[:], cols_d[:, :], [], ["colc"], "c0")
    dma("sp", slot_iota[:], slotiota_d[:, :], [], ["slot_iota"], "c0")
    dma("sp", tok_iota[:], tokiota_d[:, :], [], ["tok_iota"], "c0")
    dma("sp", rbb[:], relb_d[0:1, :].partition_broadcast(128), [], ["rbb"], "c0")
    dma("sp", wr_sb[:], wr_d.rearrange("(k p) n -> p k n", p=128), [], ["wr_sb"], "c0")
    cp("act", ident_b[:], ident_f[:], ["ident_f"], ["ident_b"])
    epsc = P.sbuf("epsc", [128, 2], F32)
    memset("pool", epsc[:, 0:1], RMS_EPS, ["epsc"])
    memset("pool", epsc[:, 1:2], LN_EPS, ["epsc"])
    memset("pool", ones_f[:], 1.0, ["ones_f"])
    memset("pool", ones_b[:], 1.0, ["ones_b"])
    memset("pool", aff_all[:], 0.0, ["aff_all"])

    steps = _band_steps()
    nst = len(steps)

    with P.scope():
        cT_sb = P.sbuf("cT_sb", [128, 16], F32)
        cact = P.sbuf("cact", [128, 16], F32)
        wa = [P.sbuf("wa%d" % i, [128, 8, 512], F32) for i in range(2)]
        mod_sb = P.sbuf("mod_sb", [2, 6 * D], F32)
        bada_sb = P.sbuf("bada_sb", [2, 6 * D], F32)
        dma("sp", cT_sb[:], cT_d[:, :], [], ["cT_sb"], "c1")
        dma("sp", bada_sb[:], bada_d[0:1, :].partition_broadcast(2), [], ["bada_sb"], "c1")
        act(cact[:], cT_sb[:], AF.Sigmoid, ["cT_sb"], ["cact"])
        tt("dve", cact[:], cact[:], cT_sb[:], ALU.mult, ["cact", "cT_sb"], ["cact"])
        wada_v = wada_d.rearrange("(k p) n -> p k n", p=128)
        bg = []
        reli = P.sbuf("reli", [128, 384], F32)
        stp = [P.sbuf("stp%d" % i, [128, 384], F32) for i in range(2)]
        dlt = P.sbuf("dlt", [128, nst * 4], F32)
        wuq_st = P.sbuf("wuq_st", [128, 2, 1536], F32)
        wukv_st = P.sbuf("wukv_st", [128, 1024], F32)
        qn_sb = P.sbuf("qn_sb", [128, 2], F32)
        kvn_sb = P.sbuf("kvn_sb", [128, 1], F32)
        lin = P.sbuf("lin", [128, 256], F32)
        lpr = P.sbuf("lpr", [128, 128], F32)
        lsum = P.sbuf("lsum", [128, 2], F32)
        sub_st = P.sbuf("sub_st", [128, 1], F32)
        dma("pool", reli[:], reliota_d[:, :], [], ["reli"], "c5")
        dma("pool", wuq_st[:], wuq_d.rearrange("(k p) n -> p k n", p=128), [], ["wuq_st"], "c3")
        dma("pool", wukv_st[:], wukv_d[:, :], [], ["wukv_st"], "c3")
        dma("pool", qn_sb[:], qn_d[:, :], [], ["qn_sb"], "c3")
        dma("pool", kvn_sb[:], kvn_d[:, :], [], ["kvn_sb"], "c3")
        dma("pool", lin[:], lam_d[0:1, :].partition_broadcast(128), [], ["lin"], "c4")
        dma("pool", sub_st[:], subln_d[:, :], [], ["sub_st"], "c4")

        def bg_band_pre():
            for t, (thr, b0, b1) in enumerate(steps):
                tt("pool", dlt[:, t * 4:(t + 1) * 4], rbb[:, b1 * 4:(b1 + 1) * 4], rbb[:, b0 * 4:(b0 + 1) * 4],
                   ALU.subtract, ["rbb"], ["dlt"])
            for h in range(4):
                ts("pool", band[:, h, 0:384], reli[:], 0.0, rbb[:, 31 * 4 + h:31 * 4 + h + 1], ALU.mult, ALU.add,
                   ["reli", "rbb"], [("band", h)])
                ts("pool", band[:, h, 768:1152], reli[:], 0.0, rbb[:, 15 * 4 + h:15 * 4 + h + 1], ALU.mult, ALU.add,
                   ["reli", "rbb"], [("band", h)])
        bg.append(bg_band_pre)

        def bg_band_step(t, thr):
            sp_ = stp[t % 2]
            sk = ("stp", t % 2)
            ts("dve", sp_[:], reli[:], float(thr), None, ALU.is_ge, None, ["reli"], [sk])
            for h in range(4):
                if t == 0:
                    ts("dve", band[:, h, 384:768], sp_[:], dlt[:, t * 4 + h:t * 4 + h + 1],
                       rbb[:, 15 * 4 + h:15 * 4 + h + 1], ALU.mult, ALU.add, [sk, "dlt", "rbb"], [("band", h)])
                else:
                    stt(band[:, h, 384:768], sp_[:], dlt[:, t * 4 + h:t * 4 + h + 1], band[:, h, 384:768],
                        ALU.mult, ALU.add, [sk, "dlt", ("band", h)], [("band", h)])
        for t, (thr, b0, b1) in enumerate(steps):
            bg.append(lambda t=t, thr=thr: bg_band_step(t, thr))

        def bg_fold():
            for k in range(2):
                act(wuq_b[:, k, :], wuq_st[:, k, :], AF.Copy, ["wuq_st", "qn_sb"], ["wuq_b"], scale=qn_sb[:, k:k + 1])
            act(wukv_b[:], wukv_st[:], AF.Copy, ["wukv_st", "kvn_sb"], ["wukv_b"], scale=kvn_sb[:, 0:1])
        bg.append(bg_fold)

        def bg_lam():
            tt("dve", lpr[:, 0:64], lin[:, 0:64], lin[:, 64:128], ALU.mult, ["lin"], ["lpr"])
            tt("dve", lpr[:, 64:128], lin[:, 128:192], lin[:, 192:256], ALU.mult, ["lin"], ["lpr"])
            P.op("dve", lambda e: e.reduce_sum(out=lsum[:, 0:1], in_=lpr[:, 0:64], axis=AX.X), ["lpr"], ["lsum"])
            P.op("dve", lambda e: e.reduce_sum(out=lsum[:, 1:2], in_=lpr[:, 64:128], axis=AX.X), ["lpr"], ["lsum"])
            act(lsum[:], lsum[:], AF.Exp, ["lsum"], ["lsum"])
            tt("dve", lamc[:, 0:1], lsum[:, 0:1], lsum[:, 1:2], ALU.subtract, ["lsum"], ["lamc"])
            ts("dve", lamc[:, 0:1], lamc[:, 0:1], LAM_INIT, None, ALU.add, None, ["lamc"], ["lamc"])
            ts("dve", lamc[:, 1:2], lamc[:, 0:1], -1.0, None, ALU.mult, None, ["lamc"], ["lamc"])
            ts("dve", sublnc[:], sub_st[:], 1.0 - LAM_INIT, None, ALU.mult, None, ["sub_st"], ["sublnc"])
        bg.append(bg_lam)
        nbg = len(bg)
        bgi = 0

        for j in range(12):
            w = wa[j % 2]
            wk = "wa%d" % (j % 2)
            dma("sp" if j % 2 == 0 else "act", w[:], wada_v[:, :, j * 512:(j + 1) * 512], [], [wk], wk)
            pb = PSP.get("m")
            for k in range(8):
                mm(ps[pb][0:2, :], cact[:, 2 * k:2 * k + 2], w[:, k, :], k == 0, k == 7,
                   ["cact", wk], [("ps", pb)])
            while bgi < (j + 1) * nbg // 12:
                bg[bgi]()
                bgi += 1
            tt("dve", mod_sb[0:2, j * 512:(j + 1) * 512], ps[pb][0:2, :], bada_sb[0:2, j * 512:(j + 1) * 512],
               ALU.add, [("ps", pb), "bada_sb"], ["mod_sb"])
        while bgi < nbg:
            bg[bgi]()
            bgi += 1
        ts("dve", mod_sb[0:2, 1024:2048], mod_sb[0:2, 1024:2048], 1.0, None, ALU.add, None, ["mod_sb"], ["mod_sb"])
        ts("dve", mod_sb[0:2, 4096:5120], mod_sb[0:2, 4096:5120], 1.0, None, ALU.add, None, ["mod_sb"], ["mod_sb"])
        dma("sp", mod_d[:, :], mod_sb[0:2, :], ["mod_sb"], ["mod_d"], "modw")
        mc_st = P.sbuf("mc_st", [16, 128], F32)
        for b in range(NB):
            dma("sp", mc_st[:], mod_d[b:b + 1, 0:2048].rearrange("o (r c) -> (o r) c", c=128),
                ["mod_d"], ["mc_st"], "mc")
            pb = PSP.get("m")
            tr(ps[pb][:, 0:16], mc_st[0:16, :], ident_f[0:16, 0:16], ["mc_st", "ident_f"], [("ps", pb)])
            cp("dve", modcol[:, b, :], ps[pb][:, 0:16], [("ps", pb)], ["modcol"])
        dma("sp", ag[:], ln1_d[0:1, :].partition_broadcast(128), [], ["ag"], "c2")
        dma("sp", ab[:], ln1_d[1:2, :].partition_broadcast(128), [], ["ab"], "c2")
        for b in range(NB):
            dma("sp", gfb[:, b, :], mod_d[b:b + 1, 5120:6144].partition_broadcast(128), ["mod_d"], ["gfb"], "c2")

        for h in range(4):
            ts("dve", band[:, h, :], band[:, h, :], rbb[:, 15 * 4 + h:15 * 4 + h + 1], None, ALU.subtract, None,
               [("band", h), "rbb"], [("band", h)])
        tt("dve", rbs[:, 0:4], rbb[:, 31 * 4:31 * 4 + 4], rbb[:, 15 * 4:15 * 4 + 4], ALU.subtract, ["rbb"], ["rbs"])
        act(ag[:], ag[:], AF.Copy, ["ag"], ["ag"], scale=ALPHA)
        act(ab[:], ab[:], AF.Copy, ["ab"], ["ab"], scale=ALPHA)
        dump("mod", mod_sb[0:2, :], [2, 6 * D], ["mod_sb"])
        dump("band", band[:, 0, :], [128, 1152], [("band", 0)])
        dump("lamc", lamc[:], [128, 8], ["lamc"])

    if lvl == 0:
        P.finish()
        return dbg_d

    win_v = win_d.rearrange("(k p) n -> p k n", p=128)
    wout_v = wout_d.rearrange("(k p) n -> p k n", p=128)

    for b in range(NB):
        with P.scope():
            bufA = P.sbuf("bufA", [128, 8, S], BF16)
            with P.scope():
                cqT = P.sbuf("cqT", [128, 2, S], BF16)
                ckvT = P.sbuf("ckvT", [128, S], BF16)
                ropeT = P.sbuf("ropeT", [128, 2, S], F32)
                krT = P.sbuf("krT", [128, S], BF16)
                dqT = P.sbuf("dqT", [128, 4, S], BF16)
                dkT = P.sbuf("dkT", [128, 4, S], BF16)
                dvx = P.sbuf("dvx", [128, NT, 512], BF16)

                R = slice(64, 96)
                with P.scope():
                    xs = P.sbuf("xs", [128, 4, D], F32)
                    wj = [P.sbuf("wj%d" % i, [128, 8, 512], BF16) for i in range(2)]
                    sq = [P.sbuf("sq%d" % i, [128, 512], F32) for i in range(2)]
                    rs = [P.sbuf("rs%d" % i, [128, 512], F32) for i in range(2)]
                    t1 = [P.sbuf("t1_%d" % i, [128, 512], F32) for i in range(2)]
                    ang, angc, rtmp = sq[0], sq[1], rs[0]
                    kang, kangc, ktmp, kpi = ("sq", 0), ("sq", 1), ("rs", 0), ("rs", 1)
                    pi_ = rs[1][:].bitcast(I32)
                    for q in range(4):
                        dma("act", pi_[q * 32:(q + 1) * 32, :], pos_d[b:b + 1, q * 512:(q + 1) * 512].partition_broadcast(32),
                            [], [kpi], "posi")
                    cp("dve", ang[:], pi_[:, :], [kpi], [kang])
                    ts("dve", ang[:], ang[:], colc[:, 0:1], None, ALU.mult, None, [kang, "colc"], [kang])
                    ts("dve", angc[:], ang[:], math.pi / 2.0, None, ALU.add, None, [kang], [kangc])
                    for which, (tab, tkey) in enumerate(((ang, kang), (angc, kangc))):
                        ts("dve", rtmp[:], tab[:], 1.0 / (2.0 * math.pi), None, ALU.mult, None, [tkey], [ktmp])
                        cp("dve", pi_[:, :], rtmp[:], [ktmp], [kpi])
                        cp("dve", rtmp[:], pi_[:, :], [kpi], [ktmp])
                        stt(tab[:], rtmp[:], -TWO_PI_HI, tab[:], ALU.mult, ALU.add, [ktmp, tkey], [tkey])
                        stt(tab[:], rtmp[:], -TWO_PI_LO, tab[:], ALU.mult, ALU.add, [ktmp, tkey], [tkey])
                        ts("dve", rtmp[:], tab[:], math.pi, -2.0 * math.pi, ALU.is_gt, ALU.mult, [tkey], [ktmp])
                        tt("dve", tab[:], tab[:], rtmp[:], ALU.add, [tkey, ktmp], [tkey])
                        ts("dve", rtmp[:], tab[:], -math.pi, 2.0 * math.pi, ALU.is_lt, ALU.mult, [tkey], [ktmp])
                        tt("dve", tab[:], tab[:], rtmp[:], ALU.add, [tkey, ktmp], [tkey])
                        if which == 0:
                            act(tab[:], tab[:], AF.Sin, [tkey, "colc"], [tkey], scale=colc[:, 1:2])
                        else:
                            act(tab[:], tab[:], AF.Sin, [tkey], [tkey])
                        for q in range(4):
                            dma("act", ropeT[R, 1 - which, q * 512:(q + 1) * 512], tab[q * 32:(q + 1) * 32, :],
                                [tkey], [("rope", 1 - which)], "ropew")
                    for g in range(4):
                        dma("sp", xs[:], x_d[b, g * 512:(g + 1) * 512, :].rearrange("(t p) d -> p t d", p=128),
                            [], ["xs"], "xs")
                        for k in range(8):
                            pb = PSP.get("s")
                            for t4 in range(4):
                                tr(ps[pb][:, t4 * 128:(t4 + 1) * 128], xs[:, t4, k * 128:(k + 1) * 128], ident_f[:],
                                   ["xs", "ident_f"], [("ps", pb)])
                            o_ap = bufA[:, k, g * 512:(g + 1) * 512]
                            if k % 2 == 0:
                                act(o_ap, ps[pb][:], AF.Identity, [("ps", pb), "modcol"], [("uT", g)],
                                    bias=modcol[:, b, k:k + 1], scale=modcol[:, b, 8 + k:9 + k])
                            else:
                                ts("dve", o_ap, ps[pb][:], modcol[:, b, 8 + k:9 + k], modcol[:, b, k:k + 1],
                                   ALU.mult, ALU.add, [("ps", pb), "modcol"], [("uT", g)])
                    dump("uT%d" % b, bufA[:, 0, :], [128, S], [("uT", g) for g in range(4)], BF16)

                    def proj_mm(pb, wt, wkey, c0, m, g):
                        for k in range(8):
                            mm(ps[pb][0:m, :], wt[:, k, c0:c0 + m], bufA[:, k, g * 512:(g + 1) * 512], k == 0, k == 7,
                               [wkey, ("uT", g)], [("ps", pb)])

                    for chunk in range(4):
                        w = wj[chunk % 2]
                        wkey = "wj%d" % (chunk % 2)
                        dma("pool", w[:], win_v[:, :, chunk * 512:(chunk + 1) * 512], [], [wkey], wkey)
                        for g in range(4):
                            G = slice(g * 512, (g + 1) * 512)
                            if chunk == 0:
                                pq = [PSP.get("a"), PSP.get("a")]
                                for c in range(2):
                                    proj_mm(pq[c], w, wkey, c * 128, 128, g)
                                    act(sq[c][:], ps[pq[c]][:], AF.Square, [("ps", pq[c])], [("sq", c)])
                                pr = PSP.get("m")
                                for c in range(2):
                                    mm(ps[pr][:], ones_f[:], sq[c][:], c == 0, c == 1, ["ones_f", ("sq", c)], [("ps", pr)])
                                act_pow(rs[0][:], ps[pr][:], [("ps", pr), "epsc"], [("rs", 0)], -0.5, scale=1.0 / 256.0, bias=epsc[:, 0:1])
                                for c in range(2):
                                    tt("dve", cqT[:, c, G], ps[pq[c]][:], rs[0][:], ALU.mult, [("ps", pq[c]), ("rs", 0)],
                                       [("cqT", g)])
                                pk = PSP.get("a")
                                proj_mm(pk, w, wkey, 256, 128, g)
                                act(sq[0][:], ps[pk][:], AF.Square, [("ps", pk)], [("sq", 0)])
                                pr = PSP.get("m")
                                mm(ps[pr][:], ones_f[:], sq[0][:], True, True, ["ones_f", ("sq", 0)], [("ps", pr)])
                                act_pow(rs[1][:], ps[pr][:], [("ps", pr), "epsc"], [("rs", 1)], -0.5, scale=1.0 / 128.0, bias=epsc[:, 0:1])
                                tt("dve", ckvT[:, G], ps[pk][:], rs[1][:], ALU.mult, [("ps", pk), ("rs", 1)], [("ckvT", g)])
                                pa = PSP.get("a")
                                pb2 = PSP.get("a")
                                proj_mm(pa, w, wkey, 320, 96, g)
                                proj_mm(pb2, w, wkey, 416, 96, g)
                                tt("dve", t1[0][R, :], ps[pa][R, :], ropeT[R, 0, G], ALU.mult,
                                   [("ps", pa), ("rope", 0)], [("t1", 0)])
                                tt("dve", t1[1][R, :], ps[pb2][R, :], ropeT[R, 1, G], ALU.mult,
                                   [("ps", pb2), ("rope", 1)], [("t1", 1)])
                                tt("pool", krT[R, G], t1[0][R, :], t1[1][R, :], ALU.add, [("t1", 0), ("t1", 1)], [("krT", g)])
                            elif chunk in (1, 2):
                                dst = dqT if chunk == 1 else dkT
                                nm = "dqT" if chunk == 1 else "dkT"
                                for h in range(4):
                                    pq_ = PSP.get("a")
                                    proj_mm(pq_, w, wkey, h * 128, 128, g)
                                    if h % 2 == 0:
                                        cp("act", dst[:, h, G], ps[pq_][:], [("ps", pq_)], [(nm, h, g)])
                                    else:
                                        cp("dve", dst[:, h, G], ps[pq_][:], [("ps", pq_)], [(nm, h, g)])
                            else:
                                for t4 in range(4):
                                    t = g * 4 + t4
                                    pv = PSP.get("a")
                                    for k in range(8):
                                        mm(ps[pv][:], bufA[:, k, t * 128:(t + 1) * 128], w[:, k, :], k == 0, k == 7,
                                           [wkey, ("uT", g)], [("ps", pv)])
                                    if t4 % 2 == 0:
                                        cp("act", dvx[:, t, :], ps[pv][:], [("ps", pv)], [("dvx", t)])
                                    else:
                                        cp("dve", dvx[:, t, :], ps[pv][:], [("ps", pv)], [("dvx", t)])
                    dump("cqT%d" % b, cqT[:, 0, :], [128, S], [("cqT", g) for g in range(4)], BF16)
                    dump("ckvT%d" % b, ckvT[:, :], [128, S], [("ckvT", g) for g in range(4)], BF16)
                    dump("krT%d" % b, krT[:, :], [128, S], [("krT", g) for g in range(4)], BF16)
                    dump("dqT%d" % b, dqT[:, 1, :], [128, S], [("dqT", 1, g) for g in range(4)], BF16)
                    dump("dvx%d" % b, dvx[:, 3, :], [128, 512], [("dvx", 3)], BF16)
                    dump("rope%d" % b, ropeT[:, 0, :], [128, S], [("rope", 0)])

                if lvl == 1:
                    continue

                with P.scope():
                    kT = [P.sbuf("kT%d" % i, [128, S], BF16) for i in range(2)]
                    qT = [P.sbuf("qT%d" % i, [128, S], BF16) for i in range(2)]
                    vh = [P.sbuf("vh%d" % i, [128, NT, 128], BF16) for i in range(2)]
                    NPT = 6
                    pT = [P.sbuf("pT%d" % i, [128, 512], BF16) for i in range(NPT)]
                    tmpb = [P.sbuf("tmpb%d" % i, [128, 512], F32) for i in range(2)]
                    osb = [P.sbuf("osb%d" % i, [128, 512], F32) for i in range(2)]
                    pctr = [0]
                    tctr = [0]
                    gctr = [0]

                    bcs = [P.sbuf("bcs%d" % i, [128, 512], F32) for i in range(2)]
                    rT = [P.sbuf("rT%d" % i, [128, 4], F32) for i in range(2)]
                    voff_of = {}
                    memset("pool", vh[0][:, :, 64:65], 1.0, [("vh", 0)])
                    memset("pool", vh[1][:, :, 0:64], 0.0, [("vh", 1)])
                    memset("pool", vh[1][:, :, 0:1], 1.0, [("vh", 1)])

                    PSP.cls = {"s": [0, 1, 2, 7], "a": [3, 4], "m": [5, 6]}

                    def mla_prep_pieces(h):
                        par = h % 2
                        kTh, qTh, vhh = kT[par], qT[par], vh[par]
                        voff = 0 if par == 0 else 64
                        pieces = []

                        def k_piece(g):
                            G = slice(g * 512, (g + 1) * 512)
                            pk = PSP.get("m")
                            mm(ps[pk][0:64, :], wukv_b[:, h * 64:(h + 1) * 64], ckvT[:, G], True, True,
                               ["wukv_b", ("ckvT", g)], [("ps", pk)])
                            cp("dve", kTh[0:64, G], ps[pk][0:64, :], [("ps", pk)], [("kT", par)])

                        def kr_piece():
                            dma("sp", kTh[R, :], krT[R, :], [("krT", g) for g in range(4)], [("kT", par)], "krc%d" % par)

                        def v_piece(half):
                            pv = PSP.get("m")
                            for t8 in range(8):
                                t = half * 8 + t8
                                mm(ps[pv][:, t8 * 64:(t8 + 1) * 64], ckvT[:, t * 128:(t + 1) * 128],
                                   wukv_b[:, 512 + h * 64:512 + (h + 1) * 64], True, True,
                                   ["wukv_b", ("ckvT", t // 4)], [("ps", pv)])
                            cp("dve", vhh[:, half * 8:(half + 1) * 8, voff:voff + 64],
                               ps[pv][:].rearrange("p (t d) -> p t d", d=64), [("ps", pv)], [("vh", par)])

                        def q_piece(g):
                            G = slice(g * 512, (g + 1) * 512)
                            pa = PSP.get("m")
                            pb2 = PSP.get("m")
                            for c in range(2):
                                mm(ps[pa][0:96, :], wuq_b[:, c, h * 192:h * 192 + 96], cqT[:, c, G], c == 0, c == 1,
                                   ["wuq_b", ("cqT", g)], [("ps", pa)])
                            for c in range(2):
                                mm(ps[pb2][0:96, :], wuq_b[:, c, h * 192 + 96:h * 192 + 192], cqT[:, c, G], c == 0, c == 1,
                                   ["wuq_b", ("cqT", g)], [("ps", pb2)])
                            cp("dve", qTh[0:64, G], ps[pa][0:64, :], [("ps", pa)], [("qT", par, g)])
                            tt("dve", tmpb[0][R, :], ps[pa][R, :], ropeT[R, 0, G], ALU.mult, [("ps", pa), ("rope", 0)], [("tmpb", 0)])
                            tt("dve", tmpb[1][R, :], ps[pb2][R, :], ropeT[R, 1, G], ALU.mult, [("ps", pb2), ("rope", 1)], [("tmpb", 1)])
                            tt("dve", qTh[R, G], tmpb[0][R, :], tmpb[1][R, :], ALU.add, [("tmpb", 0), ("tmpb", 1)], [("qT", par, g)])

                        for g in range(4):
                            pieces.append(lambda g=g: k_piece(g))
                        pieces.append(kr_piece)
                        for half in range(2):
                            pieces.append(lambda half=half: v_piece(half))
                        for g in range(4):
                            pieces.append(lambda g=g: q_piece(g))
                        return pieces

                    def mla_prep(h):
                        for pc in mla_prep_pieces(h):
                            pc()

                    prep_cache = {}

                    LOOK = 3

                    def run_pipeline(blocks, stage_A, stage_BC):
                        info = [dict() for _ in blocks]
                        deferred = []
                        nblk = len(blocks)
                        for step in range(nblk + LOOK):
                            if step < nblk:
                                stage_A(blocks, info, step)
                            j = step - LOOK
                            if j >= 0:
                                stage_BC(blocks, info, j, step, deferred)
                            while deferred and deferred[0][0] <= step:
                                deferred.pop(0)[1]()
                        while deferred:
                            deferred.pop(0)[1]()

                    def mla_A(blocks, info, i):
                        h, g, kt, st = blocks[i]
                        G = slice(g * 512, (g + 1) * 512)
                        pss = PSP.get("s")
                        info[i]["pss"] = pss
                        par = h % 2
                        key = g * NT + kt
                        if h + 1 < 8 and key >= LOOK and (key - LOOK) % 4 == 0:
                            if h + 1 not in prep_cache:
                                prep_cache[h + 1] = mla_prep_pieces(h + 1)
                            pi_ = (key - LOOK) // 4
                            if pi_ < len(prep_cache[h + 1]):
                                prep_cache[h + 1][pi_]()
                        if WARM_DUMMY:
                            mm(ps[pss][:, 0:WARM_DUMMY], ident_b[:, :], ident_b[:, 0:WARM_DUMMY], True, True,
                               ["ident_b"], [("ps", pss)])
                        mm(ps[pss][:], kT[par][0:96, kt * 128:(kt + 1) * 128], qT[par][0:96, G], True, True,
                           [("kT", par), ("qT", par, g)], [("ps", pss)])

                    def mla_BC(blocks, info, i, step, deferred):
                        h, g, kt, st = blocks[i]
                        G = slice(g * 512, (g + 1) * 512)
                        pss = info[i]["pss"]
                        pi = pctr[0] % NPT
                        pctr[0] += 1
                        par = h % 2
                        voff = 0 if par == 0 else 64
                        M = 65 if par == 0 else 128
                        act(pT[pi][:], ps[pss][:], AF.Exp, [("ps", pss)], [("pT", pi)], scale=MLA_SCALE)
                        if kt == 0:
                            st["po"] = PSP.get("a")
                        po = st["po"]
                        mm(ps[po][0:M, :], vh[par][:, kt, 0:M], pT[pi][:], kt == 0, kt == NT - 1,
                           [("vh", par), ("pT", pi)], [("ps", po)])
                        if kt == NT - 1:
                            srow = 64 if par == 0 else 0
                            ri = gctr[0] % 2
                            gctr[0] += 1
                            SR = slice(srow, srow + 1)
                            ER = slice(0, 65) if par == 0 else slice(0, 128)
                            cp("dve", osb[ri][ER, :], ps[po][ER, :], [("ps", po)], [("rrow", ri), ("osb", ri)])
                            slot = (gctr[0] - 1) % 8
                            dma("sp", rs_d[slot:slot + 1, :], osb[ri][SR, :], [("rrow", ri)], [("rs_d", slot)], "rsw%d" % ri)
                            dma("sp", rT[ri][:, :], rs_d[slot:slot + 1, :].rearrange("o (p f) -> (o p) f", f=4),
                                [("rs_d", slot)], [("rT", ri)], "rT%d" % ri)

                            def post1b(ri=ri, slot=slot):
                                recip(rT[ri][:, :], rT[ri][:, :], [("rT", ri)], [("rT", ri)])
                                dma("pool", rs2_d[slot:slot + 1, :].rearrange("o (p f) -> (o p) f", f=4), rT[ri][:, :],
                                    [("rT", ri)], [("rs2_d", slot)], "rs2w%d" % ri)
                                dma("pool", bcs[ri][voff_of[ri]:voff_of[ri] + 64, :],
                                    rs2_d[slot:slot + 1, :].partition_broadcast(64),
                                    [("rs2_d", slot)], [("bcs", ri)], "bcs%d" % ri)
                            voff_of[ri] = voff
                            deferred.append((step + 12, post1b))

                            def post2(ri=ri, voff=voff, h=h, G=G, g=g):
                                tt("dve", bufA[voff:voff + 64, h // 2, G], osb[ri][voff:voff + 64, :],
                                   bcs[ri][voff:voff + 64, :], ALU.mult, [("osb", ri), ("bcs", ri)], [("oT", g)])
                            deferred.append((step + 26, post2))
                            deferred.sort(key=lambda x: x[0])

                    mla_prep(0)
                    blocks = [(h, g, kt, st) for h in range(8) for g in range(4) for st in ({},) for kt in range(NT)]
                    run_pipeline(blocks, mla_A, mla_BC)

                with P.scope():
                    NPT = 6
                    pT = [P.sbuf("pTd%d" % i, [128, 512], BF16) for i in range(NPT)]
                    rin = [P.sbuf("rind%d" % i, [128, 512], F32) for i in range(2)]
                    dtm = [P.sbuf("dtmd%d" % i, [128, 512], F32) for i in range(6)]
                    gctr = [0]
                    dqp = [[P.sbuf("dqp%d_%d" % (i, m), [128, S], BF16) for m in range(2)] for i in range(2)]
                    pctr = [0]
                    tctr = [0]
                    for i in range(2):
                        memset("pool", dqp[i][0][64:128, :], 0.0, [("dqp", i)])
                        memset("pool", dqp[i][1][0:64, :], 0.0, [("dqp", i)])
                    PSP.cls = {"s": [0, 1, 2, 7], "a": [3, 4, 5, 6], "m": []}

                    def dif_prep(h):
                        par = h % 2
                        dma("sp", dqp[par][0][0:64, :], dqT[0:64, h, :], [("dqT", h, g) for g in range(4)], [("dqp", par)],
                            "dqc%d" % par)
                        dma("sp", dqp[par][1][64:128, :], dqT[64:128, h, :], [("dqT", h, g) for g in range(4)], [("dqp", par)],
                            "dqc%d" % par)

                    def dif_A(blocks, info, i):
                        h, g, m, kt, st = blocks[i]
                        G = slice(g * 512, (g + 1) * 512)
                        pss = PSP.get("s")
                        info[i]["pss"] = pss
                        par = h % 2
                        if g == 0 and m == 0 and kt == LOOK and h + 1 < 4:
                            dif_prep(h + 1)
                        mm(ps[pss][:], dkT[:, h, kt * 128:(kt + 1) * 128], dqp[par][m][:, G], True, True,
                           [("dkT", h, kt // 4), ("dqp", par)], [("ps", pss)])

                    def dif_BC(blocks, info, i, step, deferred):
                        h, g, m, kt, st = blocks[i]
                        G = slice(g * 512, (g + 1) * 512)
                        pss = info[i]["pss"]
                        pi = pctr[0] % NPT
                        pctr[0] += 1
                        delta = kt * 128 - g * 512
                        if -128 <= delta <= 512:
                            st0 = 512 - delta
                            stt(ps[pss][:], ps[pss][:], DIFF_SCALE, band[:, h, st0:st0 + 512], ALU.mult, ALU.add,
                                [("ps", pss), ("band", h)], [("ps", pss)])
                            act(pT[pi][:], ps[pss][:], AF.Exp, [("ps", pss)], [("pT", pi)])
                        else:
                            if delta > 0:
                                act(pT[pi][:], ps[pss][:], AF.Exp, [("ps", pss), "rbs"], [("pT", pi)],
                                    bias=rbs[:, h:h + 1], scale=DIFF_SCALE)
                            else:
                                act(pT[pi][:], ps[pss][:], AF.Exp, [("ps", pss)], [("pT", pi)], scale=DIFF_SCALE)
                        if m == 0 and kt == 0:
                            st["od"] = [PSP.get("a"), PSP.get("a")]
                            st["sb"] = [PSP.get("a"), PSP.get("a")]
                        od, sb = st["od"][m], st["sb"][m]
                        mm(ps[od][:], dvx[:, kt, h * 128:(h + 1) * 128], pT[pi][:], kt == 0, kt == NT - 1,
                           [("dvx", kt), ("pT", pi)], [("ps", od)])
                        mm(ps[sb][:], ones_b[:, :], pT[pi][:], kt == 0, kt == NT - 1,
                           ["ones_b", ("pT", pi)], [("ps", sb)])
                        if kt == NT - 1:
                            di = st.setdefault("di", gctr[0] % 2)
                            if m == 0:
                                gctr[0] += 1
                            dA, dB, dS = dtm[di * 3], dtm[di * 3 + 1], dtm[di * 3 + 2]
                            kA, kB, kS = ("dtm", di * 3), ("dtm", di * 3 + 1), ("dtm", di * 3 + 2)
                            def post1(m=m, od=od, sb=sb, h=h, G=G, g=g, dA=dA, dB=dB, dS=dS, kA=kA, kB=kB, kS=kS,
                                      step=step):
                                act_pow(rin[m][:], ps[sb][:], [("ps", sb)], [("rin", m)], -1.0)
                                if m == 0:
                                    tt("dve", dA[:], ps[od][:], rin[0][:], ALU.mult, [("ps", od), ("rin", 0)], [kA])
                                else:
                                    stt(dB[:], ps[od][:], lamc[:, 1:2], rin[1][:], ALU.mult, ALU.mult,
                                        [("ps", od), ("rin", 1), "lamc"], [kB])
                                    tt("dve", dA[:], dA[:], dB[:], ALU.add, [kA, kB], [kA])
                                    tt("dve", dS[:], dA[:], dA[:], ALU.mult, [kA], [kS])

                                    def post2d():
                                        pr = sb
                                        mm(ps[pr][:], ones_f[:], dS[:], True, True, ["ones_f", kS], [("ps", pr)])
                                        act_pow(dB[:], ps[pr][:], [("ps", pr), "epsc"], [kB], -0.5, scale=1.0 / 128.0,
                                                bias=epsc[:, 0:1])
                                        stt(bufA[:, 4 + h, G], dA[:], sublnc[:, 0:1], dB[:], ALU.mult, ALU.mult,
                                            [kA, kB, "sublnc"], [("oT", g)])
                                    deferred.append((step + 9, post2d))
                                    deferred.sort(key=lambda x: x[0])
                            deferred.append((step + 3, post1))
                            deferred.sort(key=lambda x: x[0])

                    dif_prep(0)
                    blocks = [(h, g, m, kt, st) for h in range(4) for g in range(4) for st in ({},)
                              for m in range(2) for kt in range(NT)]
                    run_pipeline(blocks, dif_A, dif_BC)
                    PSP.cls = {"s": [0, 1, 2], "a": [3, 4, 5], "m": [6, 7]}
                    for kk in (0, 1, 4, 7):
                        dump("oT%d_%d" % (kk, b), bufA[:, kk, :], [128, S], [("oT", g) for g in range(4)], BF16)

            if lvl == 2:
                continue

            with P.scope():
                wout_b = P.sbuf("wout_b", [128, 8, D], BF16)
                wst = [P.sbuf("wst%d" % i, [128, D], F32) for i in range(2)]
                gab = P.sbuf("gab", [128, D], F32)
                Gp = P.sbuf("Gp", [128, D], F32)
                Bp = P.sbuf("Bp", [128, D], F32)
                NBF = 3
                xs2 = [P.sbuf("xs2_%d" % i, [128, D], F32) for i in range(NBF)]
                pre = [P.sbuf("pre%d" % i, [128, D], F32) for i in range(NBF)]
                xn = [P.sbuf("xn%d" % i, [128, D], F32) for i in range(NBF)]
                acc = [P.sbuf("acc%d" % i, [128, D], F32) for i in range(NBF)]
                uf = [P.sbuf("uf%d" % i, [128, D], F32) for i in range(NBF)]
                ufb = [P.sbuf("ufb%d" % i, [128, D], BF16) for i in range(NBF)]
                ufT = [P.sbuf("ufT%d" % i, [128, 8, 128], F32) for i in range(NBF)]
                stats = [P.sbuf("stats%d" % i, [128, 20], F32) for i in range(NBF)]
                lg = [P.sbuf("lg%d" % i, [128, 32], F32) for i in range(NBF)]

                dma("sp", gab[:], mod_d[b:b + 1, 2048:3072].partition_broadcast(128), ["mod_d"], ["gab"], "e0")
                dma("sp", Gp[:], mod_d[b:b + 1, 4096:5120].partition_broadcast(128), ["mod_d"], ["Gp"], "e0")
                dma("sp", Bp[:], mod_d[b:b + 1, 3072:4096].partition_broadcast(128), ["mod_d"], ["Bp"], "e0")
                tt("dve", xn[0][:], ab[:], Gp[:], ALU.mult, ["ab", "Gp"], [("xn", 0)])
                stt(Bp[:], xn[0][:], 1.0 / ALPHA, Bp[:], ALU.mult, ALU.add, [("xn", 0), "Bp"], ["Bp"])
                tt("dve", xn[1][:], ag[:], Gp[:], ALU.mult, ["ag", "Gp"], [("xn", 1)])
                ts("dve", Gp[:], xn[1][:], 1.0 / ALPHA, None, ALU.mult, None, [("xn", 1)], ["Gp"])
                for k in range(8):
                    dma("sp", wst[k % 2][:], wout_v[:, k, :], [], [("wst", k % 2)], "wst%d" % (k % 2))
                    tt("dve", wout_b[:, k, :], wst[k % 2][:], gab[:], ALU.mult,
                       [("wst", k % 2), "gab"], ["wout_b"])

                def epi_s1(t):
                    i3 = t % NBF
                    g = t // 4
                    T = slice(t * 128, (t + 1) * 128)
                    dma("sp", xs2[i3][:], x_d[b, T, :], [], [("xs2", i3)], "xs2_%d" % i3)
                    for half in range(2):
                        H = slice(half * 512, (half + 1) * 512)
                        pm = PSP.get("a")
                        for k in range(8):
                            mm(ps[pm][:], bufA[:, k, T], wout_b[:, k, H], k == 0, k == 7, [("oT", g), "wout_b"], [("ps", pm)])
                        stt(pre[i3][:, H], xs2[i3][:, H], ALPHA, ps[pm][:], ALU.mult, ALU.add,
                            [("xs2", i3), ("ps", pm)], [("pre", i3)])
                        P.op("dve", lambda e, i3=i3, half=half, H=H: e.bn_stats(out=stats[i3][:, half * 6:(half + 1) * 6],
                                                                              in_=pre[i3][:, H]),
                             [("pre", i3)], [("stats", i3)])
                    P.op("dve", lambda e, i3=i3: e.bn_aggr(out=stats[i3][:, 12:14], in_=stats[i3][:, 0:12]),
                         [("stats", i3)], [("stats", i3)])
                    act_pow(stats[i3][:, 15:16], stats[i3][:, 13:14], [("stats", i3), "epsc"], [("rstd", i3)], -0.5,
                            bias=epsc[:, 1:2])

                def epi_s2(t):
                    i3 = t % NBF
                    ts("dve", stats[i3][:, 16:17], stats[i3][:, 12:13], stats[i3][:, 15:16], -1.0, ALU.mult, ALU.mult,
                       [("stats", i3), ("rstd", i3)], [("nmr", i3)])
                    act(xn[i3][:], pre[i3][:], AF.Identity, [("pre", i3), ("rstd", i3), ("nmr", i3)], [("xn", i3)],
                        bias=stats[i3][:, 16:17], scale=stats[i3][:, 15:16])

                def epi_s3(t):
                    i3 = t % NBF
                    tt("dve", uf[i3][:], xn[i3][:], Gp[:], ALU.mult, [("xn", i3), "Gp"], [("uf", i3)])
                    tt("dve", uf[i3][:], uf[i3][:], Bp[:], ALU.add, [("uf", i3), "Bp"], [("uf", i3)])
                    tt("dve", acc[i3][:], xn[i3][:], ag[:], ALU.mult, [("xn", i3), "ag"], [("acc", i3)])
                    tt("dve", acc[i3][:], acc[i3][:], ab[:], ALU.add, [("acc", i3), "ab"], [("acc", i3)])
                    dma("pool", acc_d[b * S + t * 128:b * S + (t + 1) * 128, :], acc[i3][:], [("acc", i3)], [("acc_d", b)],
                        "accw%d" % i3)
                    cp("act", ufb[i3][:], uf[i3][:], [("uf", i3)], [("ufb", i3)])
                    dma("pool", uf_d[b * S + t * 128:b * S + (t + 1) * 128, :], ufb[i3][:], [("ufb", i3)], [("uf_d", b)],
                        "ufw%d" % i3)

                def epi_s4(t):
                    i3 = t % NBF
                    for half in range(2):
                        pt_ = PSP.get("s")
                        for k4 in range(4):
                            k = half * 4 + k4
                            tr(ps[pt_][:, k4 * 128:(k4 + 1) * 128], uf[i3][:, k * 128:(k + 1) * 128], ident_f[:],
                               [("uf", i3), "ident_f"], [("ps", pt_)])
                        cp("act", ufT[i3][:, half * 4:(half + 1) * 4, :], ps[pt_][:].rearrange("p (k t) -> p k t", t=128),
                           [("ps", pt_)], [("ufT", i3)])

                def epi_s5(t):
                    i3 = t % NBF
                    pl = PSP.get("m")
                    for k in range(8):
                        mm(ps[pl][:, 0:NE], ufT[i3][:, k, :], wr_sb[:, k, :], k == 0, k == 7, [("ufT", i3), "wr_sb"],
                           [("ps", pl)])
                    P.op("dve", lambda e, i3=i3, pl=pl: e.reduce_max(out=lg[i3][:, 16:17], in_=ps[pl][:, 0:NE], axis=AX.X),
                         [("ps", pl)], [("lgm", i3)])
                    ts("dve", lg[i3][:, 17:18], lg[i3][:, 16:17], -1.0, None, ALU.mult, None, [("lgm", i3)], [("lgm", i3)])
                    act(lg[i3][:, 0:NE], ps[pl][:, 0:NE], AF.Exp, [("ps", pl), ("lgm", i3)], [("lg", i3)],
                        bias=lg[i3][:, 17:18], accum_out=lg[i3][:, 18:19])

                def epi_s6(t):
                    i3 = t % NBF
                    recip(lg[i3][:, 19:20], lg[i3][:, 18:19], [("lg", i3)], [("lgr", i3)])
                    ts("dve", aff_all[:, t, b * 32:b * 32 + NE], lg[i3][:, 0:NE], lg[i3][:, 19:20], None, ALU.mult, None,
                       [("lg", i3), ("lgr", i3)], ["aff_all"])
                    if dbg and t == 3:
                        dump("pre3_%d" % b, pre[i3][:], [128, D], [("pre", i3)])
                        dump("uf3_%d" % b, uf[i3][:], [128, D], [("uf", i3)])

                stages = [epi_s1, epi_s2, epi_s3, epi_s4, epi_s5, epi_s6]
                for step in range(NT + len(stages) - 1):
                    for si, fn in enumerate(stages):
                        t = step - si
                        if 0 <= t < NT:
                            fn(t)

        if lvl == 3:
            continue


    if lvl <= 3:
        P.finish()
        return dbg_d

    with P.scope():
        NP = 48
        posT = P.sbuf("posT", [128, 16, NP], F32)
        maskT = P.sbuf("maskT", [128, 16, NP], F32)
        tvb = P.sbuf("tvb", [128, NB, 16, 128], BF16)
        offc = P.sbuf("offc", [128, 4], F32)
        NW = 5
        PF = NW - 2
        wbuf = [P.sbuf("wbuf%d" % i, [128, 8, 1024], BF16) for i in range(NW)]

        loads = []
        for e_ in range(NE):
            for q_ in range(2):
                loads.append(("g", e_, q_))
                loads.append(("u", e_, q_))
            for q_ in range(2):
                loads.append(("d", e_, q_))
        issued = [0]

        def issue_upto(k):
            while issued[0] <= min(k, len(loads) - 1):
                j = issued[0]
                kind, e_, q_ = loads[j]
                i = j % NW
                if kind == "g":
                    src = wg_d[e_].rearrange("(k p) f -> p k f", p=128)[:, :, q_ * 1024:(q_ + 1) * 1024]
                elif kind == "u":
                    src = wu_d[e_].rearrange("(k p) f -> p k f", p=128)[:, :, q_ * 1024:(q_ + 1) * 1024]
                else:
                    src = wd_d[e_].rearrange("(k p) d -> p k d", p=128)[:, q_ * 8:(q_ + 1) * 8, :]
                dma("pool", wbuf[i][:], src, [], [("wbuf", i)], "wbuf%d" % i)
                issued[0] += 1

        def use(j0, n):
            issue_upto(j0 + PF)
            return [(j0 + t) % NW for t in range(n)]


        issue_upto(PF)
        with P.scope():
            affT = P.sbuf("affT", [NP, S], F32)
            work = P.sbuf("work", [NP, S], F32)
            maskE = P.sbuf("maskE", [NP, S], F32)
            posE = P.sbuf("posE", [NP, S], F32)
            onesE = P.sbuf("onesE", [NP, S], F32)
            m8 = P.sbuf("m8", [NP, 8], F32)
            r1 = P.sbuf("r1", [128, 16, NE], F32)
            gb = P.sbuf("gb", [128, 16, NE], BF16)
            for g in range(4):
                pa_ = PSP.get("m")
                for t4 in range(4):
                    t = g * 4 + t4
                    tr(ps[pa_][0:NP, t4 * 128:(t4 + 1) * 128], aff_all[:, t, :], ident_f[:], ["aff_all", "ident_f"],
                       [("ps", pa_)])
                cp("act", affT[:, g * 512:(g + 1) * 512], ps[pa_][0:NP, :], [("ps", pa_)], ["affT"])
            cp("pool", work[:], affT[:], ["affT"], ["work"])
            memset("pool", onesE[:], 1.0, ["onesE"])
            for r in range(CAP // 8):
                P.op("dve", lambda e: e.max(out=m8[:], in_=work[:]), ["work"], ["m8"])
                if r < CAP // 8 - 1:
                    P.op("dve", lambda e: e.match_replace(out=work[:], in_to_replace=m8[:], in_values=work[:], imm_value=-1.0),
                         ["work", "m8"], ["work"])
            ts("dve", maskE[:], affT[:], m8[:, 7:8], None, ALU.is_ge, None, ["affT", "m8"], ["maskE"])
            P.op("dve", lambda e: e.tensor_tensor_scan(out=posE[:], data0=onesE[:], data1=maskE[:], initial=-1.0,
                                                       op0=ALU.mult, op1=ALU.add), ["onesE", "maskE"], ["posE"])
            for src, dstT, nm in ((posE, posT, "posT"), (maskE, maskT, "maskT")):
                skey = "posE" if nm == "posT" else "maskE"
                for g4 in range(4):
                    pa_ = PSP.get("m")
                    for t4 in range(4):
                        t = g4 * 4 + t4
                        tr(ps[pa_][:, t4 * NP:(t4 + 1) * NP], src[0:NP, t * 128:(t + 1) * 128], ident_f[0:NP, 0:NP],
                           [skey, "ident_f"], [("ps", pa_)])
                    cp("act", dstT[:, g4 * 4:(g4 + 1) * 4, :], ps[pa_][:, 0:4 * NP].rearrange("p (t e) -> p t e", e=NP),
                       [("ps", pa_)], [nm])
            memset("pool", tvb[:], 0.0, ["tvb"])
            for bb in range(NB):
                cp("pool", tvb[:, bb, :, 0], tok_iota[:, 0:16], ["tok_iota"], ["tvb"])
                cp("pool", tvb[:, bb, :, 1], tok_iota[:, 16:32], ["tok_iota"], ["tvb"])
                av = aff_all[:, :, bb * 32:bb * 32 + NE]
                gview = tvb[:, bb, :, 2:2 + 3 * NE].rearrange("p t (e j) -> p t e j", j=3)
                cp("dve", gb[:], av, ["aff_all"], ["gb"])
                cp("dve", gview[:, :, :, 0], gb[:], ["gb"], ["tvb"])
                tt("dve", r1[:], av, gb[:], ALU.subtract, ["aff_all", "gb"], ["r1"])
                cp("dve", gb[:], r1[:], ["r1"], ["gb"])
                cp("dve", gview[:, :, :, 1], gb[:], ["gb"], ["tvb"])
                tt("dve", r1[:], r1[:], gb[:], ALU.subtract, ["r1", "gb"], ["r1"])
                cp("dve", gview[:, :, :, 2], r1[:], ["r1"], ["tvb"])
            memset("pool", offc[:, 0:2], 0.0, ["offc"])
            memset("pool", offc[:, 2:4], float(S), ["offc"])
            dump("posT", posT[:].rearrange("p t e -> p (t e)"), [128, 16 * NP], ["posT"])

        NSEL = 4
        sel = [P.sbuf("sel%d" % i, [128, 256], BF16) for i in range(NSEL)]
        Rsb = [P.sbuf("Rsb%d" % i, [128, 512], F32) for i in range(1)]
        idxf4 = [P.sbuf("idxf4_%d" % i, [128, 8], F32) for i in range(2)]
        vsb = [P.sbuf("vsb%d" % i, [128, 4, 8], F32) for i in range(2)]
        selctr = [0]

        class RouteTask:
            def __init__(self, e_):
                self.e = e_
                self.pend = []
                self.i = 0
                self.bank = None

            def step(self):
                e_ = self.e
                if self.i > 16:
                    return
                if self.i == 0:
                    self.bank = PSP.get("m")
                bank = self.bank
                for (bb, t, si) in self.pend:
                    mm(ps[bank][:, bb * 256:(bb + 1) * 256], tvb[:, bb, t, :], sel[si][:], t == 0, t == NT - 1,
                       ["tvb", ("sel", si)], [("ps", bank)])
                self.pend = []
                if self.i < 16:
                    for j in range(2):
                        q = self.i * 2 + j
                        bb, t = q // 16, q % 16
                        si = selctr[0] % NSEL
                        selctr[0] += 1
                        col = bb * 32 + e_
                        ts("dve", sel[si][:], slot_iota[:], posT[:, t, col:col + 1], maskT[:, t, col:col + 1],
                           ALU.is_equal, ALU.mult, ["slot_iota", "posT", "maskT"], [("sel", si)])
                        self.pend.append((bb, t, si))
                else:
                    ri = e_ % 2
                    cp("act", Rsb[0][:], ps[bank][:, :], [("ps", bank)], [("Rsb", 0)])
                    pb_ = PSP.get("m")
                    for j in range(4):
                        tr(ps[pb_][:, j * 128:(j + 1) * 128], Rsb[0][:, j * 128:(j + 1) * 128], ident_f[:],
                           [("Rsb", 0), "ident_f"], [("ps", pb_)])
                    v = ps[pb_][:, :].rearrange("p (j c) -> p j c", c=128)
                    f4 = idxf4[ri]
                    g0 = 2 + 3 * e_
                    vs = vsb[ri]
                    cp("act", vs[:, :, 0:2], v[:, :, 0:2], [("ps", pb_)], [("vs", ri)])
                    cp("act", vs[:, :, 2:5], v[:, :, g0:g0 + 3], [("ps", pb_)], [("vs", ri)])
                    stt(f4[:, 0:4], vs[:, :, 1], 128.0, vs[:, :, 0], ALU.mult, ALU.add, [("vs", ri)], [("f4", ri)])
                    tt("dve", f4[:, 0:4], f4[:, 0:4], offc[:, 0:4], ALU.add, [("f4", ri), "offc"], [("f4", ri)])
                    tt("dve", f4[:, 4:8], vs[:, :, 2], vs[:, :, 3], ALU.add, [("vs", ri)], [("f4g", ri)])
                    gv = g_sb[:, :].rearrange("p (b e c) -> p b e c", b=2, e=NE)[:, :, e_, :]
                    iv = idx_i[:, :].rearrange("p (b e c) -> p b e c", b=2, e=NE)[:, :, e_, :]
                    tt("dve", gv, f4[:, 4:8].rearrange("p (b c) -> p b c", c=2),
                       vs[:, :, 4].rearrange("p (b c) -> p b c", c=2),
                       ALU.add, [("f4g", ri), ("vs", ri)], [("g_sb", e_)])
                    cp("dve", iv, f4[:, 0:4].rearrange("p (b c) -> p b c", c=2), [("f4", ri)], [("idx_i", e_)])
                self.i += 1

            def run_all(self):
                while self.i <= 16:
                    self.step()

        rtasks = [RouteTask(e_) for e_ in range(NE)]
        rtasks[0].run_all()
        rtasks[1].run_all()
        if dbg:
            for e_ in range(2, NE):
                rtasks[e_].run_all()
            dump("idxi", idx_i[:], [128, 64], [("idx_i", e_) for e_ in range(NE)], I32)
            dump("gsb", g_sb[:], [128, 64], [("g_sb", e_) for e_ in range(NE)])
        if lvl == 4:
            P.finish()
            return dbg_d

        xsg = [P.sbuf("xsg%d" % i, [128, D], BF16) for i in range(8)]
        xsT = [P.sbuf("xsT%d" % i, [128, 8, 512], BF16) for i in range(2)]
        hT = P.sbuf("hT", [128, 16, 512], BF16)
        sgt = [P.sbuf("sgt%d" % i, [128, 512], F32) for i in range(2)]
        ysb = [P.sbuf("ysb%d" % i, [128, D], F32) for i in range(2)]

        def gather(e_):
            xi = e_ % 2
            for sc in range(4):
                bb, c = sc // 2, sc % 2
                col = bb * 32 + e_ * 2 + c
                gi = xi * 4 + sc
                P.op("pool", lambda e, gi=gi, col=col: e.indirect_dma_start(
                    out=xsg[gi][:], out_offset=None, in_=uf_d[:, :],
                    in_offset=bass.IndirectOffsetOnAxis(ap=idx_i[:, col:col + 1], axis=0)),
                    [("idx_i", e_), ("uf_d", 0), ("uf_d", 1)], [("xsg", gi)], dma="xsg%d" % gi)

        issue_upto(PF)
        gather(0)
        lctr = 0
        yctr = 0
        prev_scat = {0: [], 1: []}
        for e_ in range(NE):
            xi = e_ % 2
            for sc in range(4):
                gi = xi * 4 + sc
                pt_ = PSP.get("m")
                pview = ps[pt_].bitcast(BF16)
                for k in range(8):
                    tr(pview[:, k * 128:(k + 1) * 128], xsg[gi][:, k * 128:(k + 1) * 128], ident_b[:],
                       [("xsg", gi), "ident_b"], [("ps", pt_)])
                cp("act" if sc % 2 == 0 else "dve", xsT[xi][:, :, sc * 128:(sc + 1) * 128],
                   pview[:, 0:1024].rearrange("p (k t) -> p k t", t=128), [("ps", pt_)], [("xsT", xi)])
            if e_ + 1 < NE:
                gather(e_ + 1)
            for q_ in range(2):
                wgi, wui = use(lctr, 2)
                lctr += 2
                for f8 in range(8):
                    f = q_ * 8 + f8
                    if e_ + 2 < NE:
                        rtasks[e_ + 2].step()
                    pg = PSP.get("s")
                    pu = PSP.get("a")
                    for k in range(8):
                        mm(ps[pg][:], wbuf[wgi][:, k, f8 * 128:(f8 + 1) * 128], xsT[xi][:, k, :], k == 0, k == 7,
                           [("wbuf", wgi), ("xsT", xi)], [("ps", pg)])
                    for k in range(8):
                        mm(ps[pu][:], wbuf[wui][:, k, f8 * 128:(f8 + 1) * 128], xsT[xi][:, k, :], k == 0, k == 7,
                           [("wbuf", wui), ("xsT", xi)], [("ps", pu)])
                    si = f % 2
                    act(sgt[si][:], ps[pg][:], AF.Silu, [("ps", pg)], [("sgt", si)])
                    tt("dve", hT[:, f, :], sgt[si][:], ps[pu][:], ALU.mult, [("sgt", si), ("ps", pu)], [("hT", f)])
            if e_ + 2 < NE:
                rtasks[e_ + 2].run_all()
            wdi = use(lctr, 2)
            lctr += 2
            new_scat = {0: [], 1: []}
            for sc in range(4):
                bb, c = sc // 2, sc % 2
                col = bb * 32 + e_ * 2 + c
                yi = yctr % 2
                yctr += 1
                for half in range(2):
                    py = PSP.get("m")
                    for f in range(16):
                        mm(ps[py][:], hT[:, f, sc * 128:(sc + 1) * 128],
                           wbuf[wdi[f // 8]][:, f % 8, half * 512:(half + 1) * 512],
                           f == 0, f == 15, [("hT", f), ("wbuf", wdi[f // 8])], [("ps", py)])
                    stt(ysb[yi][:, half * 512:(half + 1) * 512], ps[py][:], g_sb[:, col:col + 1],
                        gfb[:, bb, half * 512:(half + 1) * 512], ALU.mult, ALU.mult,
                        [("ps", py), ("g_sb", e_), "gfb"], [("ysb", yi)])
                ref = P.op("pool", lambda e, yi=yi, col=col: e.indirect_dma_start(
                    out=acc_d[:, :], out_offset=bass.IndirectOffsetOnAxis(ap=idx_i[:, col:col + 1], axis=0),
                    in_=ysb[yi][:], in_offset=None, compute_op=ALU.add),
                    [("ysb", yi), ("idx_i", e_)], [], dma="scat%d" % yi, after=prev_scat[bb])
                new_scat[bb].append(ref)
            prev_scat = new_scat

    if lvl == 5:
        P.finish()
        return dbg_d

    with P.scope():
        g2 = P.sbuf("g2", [128, D], F32)
        b2 = P.sbuf("b2", [128, D], F32)
        dma("sp", g2[:], ln2_d[0:1, :].partition_broadcast(128), [], ["g2"], "f0")
        dma("sp", b2[:], ln2_d[1:2, :].partition_broadcast(128), [], ["b2"], "f0")
        NF = 6
        fin = [P.sbuf("fin%d" % i, [128, D], F32) for i in range(NF)]
        fo = [P.sbuf("fo%d" % i, [128, D], F32) for i in range(NF)]
        st2 = [P.sbuf("st2_%d" % i, [128, 20], F32) for i in range(NF)]

        def fin_s1(bt):
            b, t = bt // NT, bt % NT
            i3 = bt % NF
            dma("sp", fin[i3][:], acc_d[b * S + t * 128:b * S + (t + 1) * 128, :], [("acc_d", b)], [("fin", i3)],
                "fin%d" % i3)
            for half in range(2):
                P.op("dve", lambda e, i3=i3, half=half: e.bn_stats(out=st2[i3][:, half * 6:(half + 1) * 6],
                                                                   in_=fin[i3][:, half * 512:(half + 1) * 512]),
                     [("fin", i3)], [("st2", i3)])
            P.op("dve", lambda e, i3=i3: e.bn_aggr(out=st2[i3][:, 12:14], in_=st2[i3][:, 0:12]), [("st2", i3)], [("st2", i3)])
            act_pow(st2[i3][:, 15:16], st2[i3][:, 13:14], [("st2", i3), "epsc"], [("rstd2", i3)], -0.5, bias=epsc[:, 1:2])

        def fin_s2(bt):
            i3 = bt % NF
            ts("dve", st2[i3][:, 16:17], st2[i3][:, 12:13], st2[i3][:, 15:16], -1.0, ALU.mult, ALU.mult,
               [("st2", i3), ("rstd2", i3)], [("nmr2", i3)])
            act(fo[i3][:], fin[i3][:], AF.Identity, [("fin", i3), ("rstd2", i3), ("nmr2", i3)], [("fo", i3)],
                bias=st2[i3][:, 16:17], scale=st2[i3][:, 15:16])

        def fin_s3(bt):
            b, t = bt // NT, bt % NT
            i3 = bt % NF
            tt("dve", fo[i3][:], fo[i3][:], g2[:], ALU.mult, [("fo", i3), "g2"], [("fo", i3)])
            tt("dve", fo[i3][:], fo[i3][:], b2[:], ALU.add, [("fo", i3), "b2"], [("fo", i3)])
            dma("pool", out_d[b, t * 128:(t + 1) * 128, :], fo[i3][:], [("fo", i3)], ["out_d"], "outw%d" % i3)

        NTT = NB * NT
        for step in range(NTT + 2):
            if step < NTT:
                fin_s1(step)
            if 0 <= step - 1 < NTT:
                fin_s2(step - 1)
            if 0 <= step - 2 < NTT:
                fin_s3(step - 2)
    P.finish()
    return dbg_d


def _host_inputs(inp):
    f = lambda k: np.ascontiguousarray(np.asarray(inp[k], dtype=np.float32))
    w_in = f("w_in")[0]
    kr = w_in[:, 384:416]
    kr_sw = np.concatenate([kr[:, 16:32], kr[:, 0:16]], axis=1)
    w_in_ext = np.concatenate([w_in[:, 0:416], w_in[:, 320:384], kr_sw, w_in[:, 416:1952]], axis=1)
    assert w_in_ext.shape == (1024, 2048)
    w_uq = f("w_uq")[0].reshape(256, 8, 96)
    nope, rp = w_uq[:, :, 0:64], w_uq[:, :, 64:96]
    rp_sw = np.concatenate([rp[:, :, 16:32], rp[:, :, 0:16]], axis=2)
    w_uq_ext = np.concatenate([nope, rp, nope, rp_sw], axis=2).reshape(256, 1536)
    w_ukv = f("w_ukv")[0].reshape(128, 8, 128)
    w_ukv_r = np.concatenate([w_ukv[:, :, 0:64].reshape(128, 512), w_ukv[:, :, 64:128].reshape(128, 512)], axis=1)
    qn_col = f("mla_q_norm")[0].reshape(2, 128).T
    kvn_col = f("mla_kv_norm")[0].reshape(128, 1)
    lam_in = np.concatenate([f("diff_lq1")[0], f("diff_lk1")[0], f("diff_lq2")[0], f("diff_lk2")[0]]).reshape(1, 256)
    subln_col = f("diff_subln")[0].reshape(128, 1)
    ln1 = np.stack([f("ln1_g")[0], f("ln1_b")[0]])
    ln2 = np.stack([f("ln2_g")[0], f("ln2_b")[0]])
    ident = np.eye(128, dtype=np.float32)
    kl = np.arange(128, dtype=np.float32)[:, None]
    rel_iota = (kl - np.arange(384, dtype=np.float32)[None, :] + 128.0).astype(np.float32)
    freqs = (np.float32(10000.0) ** (-np.arange(16, dtype=np.float32) / np.float32(16))).astype(np.float32)
    cols = np.zeros((128, 4), np.float32)
    for p in range(128):
        cols[p, 0] = freqs[p % 16]
        cols[p, 1] = -1.0 if (p % 32) < 16 else 1.0
    slot_iota = np.tile(np.arange(256, dtype=np.float32)[None, :], (128, 1))
    tok_iota = np.concatenate([np.tile(np.arange(128, dtype=np.float32)[:, None], (1, 16)),
                               np.tile(np.arange(16, dtype=np.float32)[None, :], (128, 1))], axis=1).astype(np.float32)
    shared = {
        "rel_bias": f("rel_bias").reshape(1, 128),
        "w_ada": f("w_ada")[0], "b_ada": f("b_ada").reshape(1, 6144),
        "w_in_ext": np.ascontiguousarray(w_in_ext), "w_uq_ext": np.ascontiguousarray(w_uq_ext),
        "w_ukv_r": np.ascontiguousarray(w_ukv_r), "qn_col": np.ascontiguousarray(qn_col), "kvn_col": kvn_col,
        "lam_in": lam_in, "subln_col": subln_col, "w_out": f("w_out")[0], "ln1": ln1,
        "w_router": f("w_router")[0], "w_gate": f("w_gate")[0], "w_up": f("w_up")[0], "w_down": f("w_down")[0],
        "ln2": ln2, "ident": ident, "rel_iota": rel_iota, "cols": cols, "slot_iota": slot_iota, "tok_iota": tok_iota,
    }
    x = f("x")
    c = f("c")
    pos = np.ascontiguousarray(np.asarray(inp["positions"], dtype=np.int32))
    maps = []
    for core in range(8):
        m = dict(shared)
        m["x"] = np.ascontiguousarray(x[core * NB:(core + 1) * NB])
        cc = c[core * NB:(core + 1) * NB]
        m["cT"] = np.ascontiguousarray(cc.reshape(NB, 8, 128).transpose(2, 1, 0).reshape(128, 16))
        m["pos"] = np.ascontiguousarray(pos[core * NB:(core + 1) * NB])
        maps.append(m)
    return maps


def _build_nc(stage="full", dbg=False):
    P1 = Prog(None)
    build(P1, stage, dbg)
    nc = bass.Bass("TRN2", target_bir_lowering=False)
    P2 = Prog(nc, sig=P1.need)
    with P2.stack[0]:
        dbg_d = build(P2, stage, dbg)
    return nc, P2, dbg_d


def kernel(**inputs):
    stage = os.environ.get("KSTAGE", "full")
    dbg = os.environ.get("KDEBUG", "0") == "1"
    ncores = int(os.environ.get("KCORES", "8"))
    nc, P2, dbg_d = _build_nc(stage, dbg)
    maps = _host_inputs(inputs)[:ncores]
    res = run_bass_kernel_spmd(nc, maps, core_ids=list(range(ncores)))
    if dbg:
        kernel.last = res.results
    outs = [r["out"] for r in res.results]
    while len(outs) < 8:
        outs.append(np.zeros_like(outs[0]))
    return np.concatenate(outs, axis=0).astype(np.float32)
```

```python
import math
import os
from contextlib import ExitStack, contextmanager

import numpy as np
import concourse.bass as bass
import concourse.mybir as mybir
from concourse.bass_utils import run_bass_kernel_spmd

F32 = mybir.dt.float32
BF16 = mybir.dt.bfloat16
I32 = mybir.dt.int32
AF = mybir.ActivationFunctionType
ALU = mybir.AluOpType
AX = mybir.AxisListType

S = 2048
D = 1024
NT = 16
NB = 2
NE = 16
FF = 2048
CAP = 256
ALPHA = 2.0 ** 0.25
MLA_SCALE = 1.0 / math.sqrt(96.0)
DIFF_SCALE = 0.125
LAM_INIT = 0.8 - 0.6 * math.exp(0.0)
LN_EPS = 1e-5
RMS_EPS = 1e-6
TWO_PI_HI = 6.28125
TWO_PI_LO = 2.0 * math.pi - 6.28125
ENG = ("pe", "act", "dve", "pool", "sp")
STRICT_SAME_ENGINE = True
WARM_DUMMY = 0


class Dummy:
    def __getattr__(self, k):
        return self

    def __getitem__(self, k):
        return self

    def __call__(self, *a, **k):
        return self


class Prog:
    def __init__(self, nc, sig=None):
        self.nc = nc
        self.dry = nc is None
        self.sig = sig if sig is not None else set()
        self.need = set()
        self.nops = {e: 0 for e in ENG}
        self.sigcount = {e: 0 for e in ENG}
        self.sigval = {}
        self.res = {}
        self.waited_eng = {e: {} for e in ENG}
        self.waited_dma = {e: {} for e in ENG}
        self.dcount = {}
        self.dsem = {}
        self.stack = [ExitStack()]
        self.esem = {}
        self.ninst = 0
        self._last_compute = {}
        if not self.dry:
            self.engobj = {"pe": nc.tensor, "act": nc.scalar, "dve": nc.vector,
                           "pool": nc.gpsimd, "sp": nc.sync}
            for e in ("pe", "act", "dve", "pool"):
                self.esem[e] = self.stack[0].enter_context(nc.semaphore("sem_" + e))

    def sbuf(self, name, shape, dtype):
        if self.dry:
            return Dummy()
        self.nalloc = getattr(self, "nalloc", 0) + 1
        return self.stack[-1].enter_context(self.nc.sbuf_tensor("%s_%d" % (name, self.nalloc), list(shape), dtype))

    def psum(self, name, shape, dtype):
        if self.dry:
            return Dummy()
        return self.stack[-1].enter_context(self.nc.psum_tensor(name, list(shape), dtype))

    def dram(self, name, shape, dtype, kind="Internal"):
        if self.dry:
            return Dummy()
        return self.nc.dram_tensor(name, list(shape), dtype, kind=kind).ap()

    @contextmanager
    def scope(self):
        self.stack.append(ExitStack())
        try:
            yield
        finally:
            self.barrier()
            self.stack.pop().close()

    def _wait(self, eng, ref):
        if ref[0] == "eng":
            _, pe, idx = ref
            w = self.waited_eng[eng]
            if w.get(pe, -1) >= idx:
                return
            w[pe] = idx
            if self.dry:
                self.need.add((pe, idx))
            else:
                self.engobj[eng].wait_ge(self.esem[pe], self.sigval[(pe, idx)])
        else:
            _, key, _cnt = ref
            cnt = self.dcount[key]
            w = self.waited_dma[eng]
            if w.get(key, 0) >= cnt:
                return
            w[key] = cnt
            if not self.dry:
                self.engobj[eng].wait_ge(self.dsem[key], 16 * cnt)

    def op(self, eng, fn, reads=(), writes=(), dma=None, after=()):
        deps = []
        for ref in after:
            self._wait(eng, ref)
        for k in reads:
            r = self.res.get(k)
            if r is not None and r[0] is not None:
                deps.append((r[0], True))
        for k in writes:
            r = self.res.get(k)
            if r is not None:
                if r[0] is not None:
                    deps.append((r[0], False))
                for rr in r[1].values():
                    deps.append((rr, False))
        for ref, raw in deps:
            if ref[0] == "eng" and dma is None and ref[1] == eng:
                if eng == "pe" or (not raw and not STRICT_SAME_ENGINE):
                    continue
            self._wait(eng, ref)
        idx = self.nops[eng]
        self.nops[eng] += 1
        if dma is not None:
            self.dcount[dma] = self.dcount.get(dma, 0) + 1
            me = ("dma", dma, self.dcount[dma])
            if not self.dry:
                if dma not in self.dsem:
                    self.dsem[dma] = self.stack[0].enter_context(
                        self.nc.semaphore("dsem_%d" % len(self.dsem)))
                ins = fn(self.engobj[eng])
                ins.then_inc(self.dsem[dma], 16)
                self.ninst += 1
        else:
            me = ("eng", eng, idx)
            self._last_compute[eng] = idx
            if not self.dry:
                ins = fn(self.engobj[eng])
                self.ninst += 1
                if (eng, idx) in self.sig:
                    self.sigcount[eng] += 1
                    ins.then_inc(self.esem[eng], 1)
                    self.sigval[(eng, idx)] = self.sigcount[eng]
        for k in writes:
            self.res[k] = [me, {}]
        for k in reads:
            r = self.res.get(k)
            if r is None:
                r = [None, {}]
                self.res[k] = r
            r[1][(me[0], me[1])] = me
        return me

    def barrier(self):
        for f in ENG:
            for e in ("pe", "act", "dve", "pool"):
                li = self._last_compute.get(e)
                if li is None or e == f:
                    continue
                self._wait(f, ("eng", e, li))
            for key in list(self.dcount.keys()):
                self._wait(f, ("dma", key, self.dcount[key]))

    def finish(self):
        for e in ("pe", "act", "dve", "pool"):
            li = self._last_compute.get(e)
            if li is not None:
                self._wait("sp", ("eng", e, li))
        for key in list(self.dcount.keys()):
            self._wait("sp", ("dma", key, self.dcount[key]))


def _t5_bucket_table_np(lo, hi):
    rel = np.arange(lo, hi + 1, dtype=np.int32)
    ret = np.where(rel > 0, 16, 0)
    n = np.abs(rel)
    nf = np.maximum(n, 1).astype(np.float32)
    a = (nf / np.float32(8)).astype(np.float32)
    lg = np.log(a).astype(np.float32)
    r = (lg / np.float32(math.log(128 / 8))).astype(np.float32)
    v = (r * np.float32(8)).astype(np.float32)
    large = np.minimum(8 + v.astype(np.int32), 15)
    return np.asarray(ret + np.where(n < 8, n, large))


def _t5_bucket_table(lo, hi):
    try:
        import jax
        import jax.numpy as jnp
        nb = 16
        max_exact = 8
        with jax.default_device(jax.devices("cpu")[0]):
            rel = jnp.arange(lo, hi + 1, dtype=jnp.int32)
            ret = jnp.where(rel > 0, nb, 0)
            n = jnp.abs(rel)
            nf = jnp.maximum(n, 1).astype(jnp.float32)
            large = max_exact + (jnp.log(nf / max_exact) / math.log(128 / max_exact)
                                 * (nb - max_exact)).astype(jnp.int32)
            large = jnp.minimum(large, nb - 1)
            out = ret + jnp.where(n < max_exact, n, large)
            return np.asarray(out)
    except Exception:
        return _t5_bucket_table_np(lo, hi)


_BUCKETS = None


def _band_steps():
    global _BUCKETS
    if _BUCKETS is None:
        _BUCKETS = _t5_bucket_table(-255, 255)
    tb = _BUCKETS
    steps = []
    for i in range(1, len(tb)):
        if tb[i] != tb[i - 1]:
            rel = -255 + i
            steps.append((rel - 0.5, int(tb[i - 1]), int(tb[i])))
    assert tb[0] == 15 and tb[-1] == 31
    assert tb[255 - 128] == 15 and tb[255 + 128] == 31
    return steps


def build(P, stage="full", dbg=False):
    STG = ["setup", "proj", "attn", "epi", "route", "moe", "full"]
    lvl = STG.index(stage)

    EI = "ExternalInput"
    x_d = P.dram("x", [NB, S, D], F32, EI)
    cT_d = P.dram("cT", [128, 16], F32, EI)
    pos_d = P.dram("pos", [NB, S], I32, EI)
    relb_d = P.dram("rel_bias", [1, 128], F32, EI)
    wada_d = P.dram("w_ada", [D, 6 * D], F32, EI)
    bada_d = P.dram("b_ada", [1, 6 * D], F32, EI)
    win_d = P.dram("w_in_ext", [D, 2048], F32, EI)
    wuq_d = P.dram("w_uq_ext", [256, 1536], F32, EI)
    wukv_d = P.dram("w_ukv_r", [128, 1024], F32, EI)
    qn_d = P.dram("qn_col", [128, 2], F32, EI)
    kvn_d = P.dram("kvn_col", [128, 1], F32, EI)
    lam_d = P.dram("lam_in", [1, 256], F32, EI)
    subln_d = P.dram("subln_col", [128, 1], F32, EI)
    wout_d = P.dram("w_out", [D, D], F32, EI)
    ln1_d = P.dram("ln1", [2, D], F32, EI)
    wr_d = P.dram("w_router", [D, NE], F32, EI)
    wg_d = P.dram("w_gate", [NE, D, FF], F32, EI)
    wu_d = P.dram("w_up", [NE, D, FF], F32, EI)
    wd_d = P.dram("w_down", [NE, FF, D], F32, EI)
    ln2_d = P.dram("ln2", [2, D], F32, EI)
    ident_d = P.dram("ident", [128, 128], F32, EI)
    reliota_d = P.dram("rel_iota", [128, 384], F32, EI)
    cols_d = P.dram("cols", [128, 4], F32, EI)
    slotiota_d = P.dram("slot_iota", [128, 256], F32, EI)
    tokiota_d = P.dram("tok_iota", [128, 32], F32, EI)
    out_d = P.dram("out", [NB, S, D], F32, "ExternalOutput")
    mod_d = P.dram("mod_scr", [NB, 6 * D], F32)
    uf_d = P.dram("uf_scr", [NB * S, D], BF16)
    acc_d = P.dram("acc_scr", [NB * S, D], F32)
    rs_d = P.dram("rs_scr", [8, 512], F32)
    rs2_d = P.dram("rs2_scr", [8, 512], F32)
    dbg_d = {}

    def dbg_out(name, shape, dtype=F32):
        if name not in dbg_d:
            dbg_d[name] = P.dram("dbg_" + name, shape, dtype, "ExternalOutput")
        return dbg_d[name]

    ident_f = P.sbuf("ident_f", [128, 128], F32)
    ident_b = P.sbuf("ident_b", [128, 128], BF16)
    ones_f = P.sbuf("ones_f", [128, 128], F32)
    ones_b = P.sbuf("ones_b", [128, 128], BF16)
    wuq_b = P.sbuf("wuq_b", [128, 2, 1536], BF16)
    wukv_b = P.sbuf("wukv_b", [128, 1024], BF16)
    band = P.sbuf("band", [128, 4, 1152], F32)
    rbb = P.sbuf("rbb", [128, 128], F32)
    rbs = P.sbuf("rbs", [128, 4], F32)
    lamc = P.sbuf("lamc", [128, 8], F32)
    sublnc = P.sbuf("sublnc", [128, 1], F32)
    colc = P.sbuf("colc", [128, 4], F32)
    modcol = P.sbuf("modcol", [128, NB, 16], F32)
    ag = P.sbuf("ag", [128, D], F32)
    ab = P.sbuf("ab", [128, D], F32)
    gfb = P.sbuf("gfb", [128, NB, D], F32)
    slot_iota = P.sbuf("slot_iota_sb", [128, 256], F32)
    tok_iota = P.sbuf("tok_iota_sb", [128, 32], F32)
    idx_i = P.sbuf("idx_i", [128, 64], I32)
    g_sb = P.sbuf("g_sb", [128, 64], F32)
    aff_all = P.sbuf("aff_all", [128, 16, 48], F32)
    wr_sb = P.sbuf("wr_sb", [128, 8, NE], F32)
    ps = [P.psum("ps%d" % i, [128, 512], F32) for i in range(8)]

    class PSPool:
        def __init__(self):
            self.cls = {"s": [0, 1, 2], "a": [3, 4, 5], "m": [6, 7]}
            self.ctr = {k: 0 for k in self.cls}

        def get(self, c):
            lst = self.cls[c]
            i = lst[self.ctr[c] % len(lst)]
            self.ctr[c] += 1
            return i

    PSP = PSPool()

    def dma(eng, out, in_, reads, writes, key):
        P.op(eng, lambda e: e.dma_start(out=out, in_=in_), reads, writes, dma=key)

    def mm(out, lhsT, rhs, start, stop, reads, writes):
        P.op("pe", lambda e: e.matmul(out, lhsT=lhsT, rhs=rhs, start=start, stop=stop), reads, writes)

    def tr(out, in_, ident, reads, writes):
        P.op("pe", lambda e: e.transpose(out=out, in_=in_, identity=ident), reads, writes)

    def act(out, in_, func, reads, writes, bias=None, scale=None, accum_out=None):
        kw = {}
        if bias is not None:
            kw["bias"] = bias
        if scale is not None:
            kw["scale"] = scale
        if accum_out is not None:
            kw["accum_out"] = accum_out
        P.op("act", lambda e: e.activation(out=out, in_=in_, func=func, **kw), reads, writes)

    def ts(eng, out, in0, s1, s2, op0, op1, reads, writes):
        if op1 is None:
            P.op(eng, lambda e: e.tensor_scalar(out=out, in0=in0, scalar1=s1, scalar2=None, op0=op0),
                 reads, writes)
        else:
            P.op(eng, lambda e: e.tensor_scalar(out=out, in0=in0, scalar1=s1, scalar2=s2, op0=op0, op1=op1),
                 reads, writes)

    def tt(eng, out, in0, in1, op, reads, writes):
        P.op(eng, lambda e: e.tensor_tensor(out=out, in0=in0, in1=in1, op=op), reads, writes)

    def stt(out, in0, scalar, in1, op0, op1, reads, writes):
        P.op("dve", lambda e: e.scalar_tensor_tensor(out=out, in0=in0, scalar=scalar, in1=in1, op0=op0, op1=op1),
             reads, writes)

    def cp(eng, out, in_, reads, writes):
        if eng == "act":
            P.op("act", lambda e: e.activation(out=out, in_=in_, func=AF.Copy), reads, writes)
        else:
            P.op(eng, lambda e: e.tensor_copy(out=out, in_=in_), reads, writes)

    def act_pow(out, in_, reads, writes, power, scale=None, bias=None):
        kw = {}
        if scale is not None:
            kw["scale"] = scale
        if bias is not None:
            kw["bias"] = bias
        P.op("act", lambda e: e.activation(out=out, in_=in_, func=AF.Ln, **kw), reads, writes)
        P.op("act", lambda e: e.activation(out=out, in_=out, func=AF.Exp, scale=float(power)), writes, writes)

    def recip(out, in_, reads, writes):
        P.op("dve", lambda e: e.reciprocal(out=out, in_=in_), reads, writes)

    def memset(eng, ap, val, writes):
        P.op(eng, lambda e: e.memset(ap, val), [], writes)

    def dump(name, src_ap, shape, reads, dtype=F32):
        if not dbg:
            return
        d = dbg_out(name, shape, dtype)
        dma("sp", d, src_ap, reads, [], "dbg")

    dma("sp", ident_f[:], ident_d[:, :], [], ["ident_f"], "c0")
    dma("sp", colc[:], cols_d[:, :], [], ["colc"], "c0")
    dma("sp", slot_iota[:], slotiota_d[:, :], [], ["slot_iota"], "c0")
    dma("sp", tok_iota[:], tokiota_d[:, :], [], ["tok_iota"], "c0")
    dma("sp", rbb[:], relb_d[0:1, :].partition_broadcast(128), [], ["rbb"], "c0")
    dma("sp", wr_sb[:], wr_d.rearrange("(k p) n -> p k n", p=128), [], ["wr_sb"], "c0")
    cp("act", ident_b[:], ident_f[:], ["ident_f"], ["ident_b"])
    epsc = P.sbuf("epsc", [128, 2], F32)
    memset("pool", epsc[:, 0:1], RMS_EPS, ["epsc"])
    memset("pool", epsc[:, 1:2], LN_EPS, ["epsc"])
    memset("pool", ones_f[:], 1.0, ["ones_f"])
    memset("pool", ones_b[:], 1.0, ["ones_b"])
    memset("pool", aff_all[:], 0.0, ["aff_all"])

    steps = _band_steps()
    nst = len(steps)

    with P.scope():
        cT_sb = P.sbuf("cT_sb", [128, 16], F32)
        cact = P.sbuf("cact", [128, 16], F32)
        wa = [P.sbuf("wa%d" % i, [128, 8, 512], F32) for i in range(2)]
        mod_sb = P.sbuf("mod_sb", [2, 6 * D], F32)
        bada_sb = P.sbuf("bada_sb", [2, 6 * D], F32)
        dma("sp", cT_sb[:], cT_d[:, :], [], ["cT_sb"], "c1")
        dma("sp", bada_sb[:], bada_d[0:1, :].partition_broadcast(2), [], ["bada_sb"], "c1")
        act(cact[:], cT_sb[:], AF.Sigmoid, ["cT_sb"], ["cact"])
        tt("dve", cact[:], cact[:], cT_sb[:], ALU.mult, ["cact", "cT_sb"], ["cact"])
        wada_v = wada_d.rearrange("(k p) n -> p k n", p=128)
        bg = []
        reli = P.sbuf("reli", [128, 384], F32)
        stp = [P.sbuf("stp%d" % i, [128, 384], F32) for i in range(2)]
        dlt = P.sbuf("dlt", [128, nst * 4], F32)
        wuq_st = P.sbuf("wuq_st", [128, 2, 1536], F32)
        wukv_st = P.sbuf("wukv_st", [128, 1024], F32)
        qn_sb = P.sbuf("qn_sb", [128, 2], F32)
        kvn_sb = P.sbuf("kvn_sb", [128, 1], F32)
        lin = P.sbuf("lin", [128, 256], F32)
        lpr = P.sbuf("lpr", [128, 128], F32)
        lsum = P.sbuf("lsum", [128, 2], F32)
        sub_st = P.sbuf("sub_st", [128, 1], F32)
        dma("pool", reli[:], reliota_d[:, :], [], ["reli"], "c5")
        dma("pool", wuq_st[:], wuq_d.rearrange("(k p) n -> p k n", p=128), [], ["wuq_st"], "c3")
        dma("pool", wukv_st[:], wukv_d[:, :], [], ["wukv_st"], "c3")
        dma("pool", qn_sb[:], qn_d[:, :], [], ["qn_sb"], "c3")
        dma("pool", kvn_sb[:], kvn_d[:, :], [], ["kvn_sb"], "c3")
        dma("pool", lin[:], lam_d[0:1, :].partition_broadcast(128), [], ["lin"], "c4")
        dma("pool", sub_st[:], subln_d[:, :], [], ["sub_st"], "c4")

        def bg_band_pre():
            for t, (thr, b0, b1) in enumerate(steps):
                tt("pool", dlt[:, t * 4:(t + 1) * 4], rbb[:, b1 * 4:(b1 + 1) * 4], rbb[:, b0 * 4:(b0 + 1) * 4],
                   ALU.subtract, ["rbb"], ["dlt"])
            for h in range(4):
                ts("pool", band[:, h, 0:384], reli[:], 0.0, rbb[:, 31 * 4 + h:31 * 4 + h + 1], ALU.mult, ALU.add,
                   ["reli", "rbb"], [("band", h)])
                ts("pool", band[:, h, 768:1152], reli[:], 0.0, rbb[:, 15 * 4 + h:15 * 4 + h + 1], ALU.mult, ALU.add,
                   ["reli", "rbb"], [("band", h)])
        bg.append(bg_band_pre)

        def bg_band_step(t, thr):
            sp_ = stp[t % 2]
            sk = ("stp", t % 2)
            ts("dve", sp_[:], reli[:], float(thr), None, ALU.is_ge, None, ["reli"], [sk])
            for h in range(4):
                if t == 0:
                    ts("dve", band[:, h, 384:768], sp_[:], dlt[:, t * 4 + h:t * 4 + h + 1],
                       rbb[:, 15 * 4 + h:15 * 4 + h + 1], ALU.mult, ALU.add, [sk, "dlt", "rbb"], [("band", h)])
                else:
                    stt(band[:, h, 384:768], sp_[:], dlt[:, t * 4 + h:t * 4 + h + 1], band[:, h, 384:768],
                        ALU.mult, ALU.add, [sk, "dlt", ("band", h)], [("band", h)])
        for t, (thr, b0, b1) in enumerate(steps):
            bg.append(lambda t=t, thr=thr: bg_band_step(t, thr))

        def bg_fold():
            for k in range(2):
                act(wuq_b[:, k, :], wuq_st[:, k, :], AF.Copy, ["wuq_st", "qn_sb"], ["wuq_b"], scale=qn_sb[:, k:k + 1])
            act(wukv_b[:], wukv_st[:], AF.Copy, ["wukv_st", "kvn_sb"], ["wukv_b"], scale=kvn_sb[:, 0:1])
        bg.append(bg_fold)

        def bg_lam():
            tt("dve", lpr[:, 0:64], lin[:, 0:64], lin[:, 64:128], ALU.mult, ["lin"], ["lpr"])
            tt("dve", lpr[:, 64:128], lin[:, 128:192], lin[:, 192:256], ALU.mult, ["lin"], ["lpr"])
            P.op("dve", lambda e: e.reduce_sum(out=lsum[:, 0:1], in_=lpr[:, 0:64], axis=AX.X), ["lpr"], ["lsum"])
            P.op("dve", lambda e: e.reduce_sum(out=lsum[:, 1:2], in_=lpr[:, 64:128], axis=AX.X), ["lpr"], ["lsum"])
            act(lsum[:], lsum[:], AF.Exp, ["lsum"], ["lsum"])
            tt("dve", lamc[:, 0:1], lsum[:, 0:1], lsum[:, 1:2], ALU.subtract, ["lsum"], ["lamc"])
            ts("dve", lamc[:, 0:1], lamc[:, 0:1], LAM_INIT, None, ALU.add, None, ["lamc"], ["lamc"])
            ts("dve", lamc[:, 1:2], lamc[:, 0:1], -1.0, None, ALU.mult, None, ["lamc"], ["lamc"])
            ts("dve", sublnc[:], sub_st[:], 1.0 - LAM_INIT, None, ALU.mult, None, ["sub_st"], ["sublnc"])
        bg.append(bg_lam)
        nbg = len(bg)
        bgi = 0

        for j in range(12):
            w = wa[j % 2]
            wk = "wa%d" % (j % 2)
            dma("sp" if j % 2 == 0 else "act", w[:], wada_v[:, :, j * 512:(j + 1) * 512], [], [wk], wk)
            pb = PSP.get("m")
            for k in range(8):
                mm(ps[pb][0:2, :], cact[:, 2 * k:2 * k + 2], w[:, k, :], k == 0, k == 7,
                   ["cact", wk], [("ps", pb)])
            while bgi < (j + 1) * nbg // 12:
                bg[bgi]()
                bgi += 1
            tt("dve", mod_sb[0:2, j * 512:(j + 1) * 512], ps[pb][0:2, :], bada_sb[0:2, j * 512:(j + 1) * 512],
               ALU.add, [("ps", pb), "bada_sb"], ["mod_sb"])
        while bgi < nbg:
            bg[bgi]()
            bgi += 1
        ts("dve", mod_sb[0:2, 1024:2048], mod_sb[0:2, 1024:2048], 1.0, None, ALU.add, None, ["mod_sb"], ["mod_sb"])
        ts("dve", mod_sb[0:2, 4096:5120], mod_sb[0:2, 4096:5120], 1.0, None, ALU.add, None, ["mod_sb"], ["mod_sb"])
        dma("sp", mod_d[:, :], mod_sb[0:2, :], ["mod_sb"], ["mod_d"], "modw")
        mc_st = P.sbuf("mc_st", [16, 128], F32)
        for b in range(NB):
            dma("sp", mc_st[:], mod_d[b:b + 1, 0:2048].rearrange("o (r c) -> (o r) c", c=128),
                ["mod_d"], ["mc_st"], "mc")
            pb = PSP.get("m")
            tr(ps[pb][:, 0:16], mc_st[0:16, :], ident_f[0:16, 0:16], ["mc_st", "ident_f"], [("ps", pb)])
            cp("dve", modcol[:, b, :], ps[pb][:, 0:16], [("ps", pb)], ["modcol"])
        dma("sp", ag[:], ln1_d[0:1, :].partition_broadcast(128), [], ["ag"], "c2")
        dma("sp", ab[:], ln1_d[1:2, :].partition_broadcast(128), [], ["ab"], "c2")
        for b in range(NB):
            dma("sp", gfb[:, b, :], mod_d[b:b + 1, 5120:6144].partition_broadcast(128), ["mod_d"], ["gfb"], "c2")

        for h in range(4):
            ts("dve", band[:, h, :], band[:, h, :], rbb[:, 15 * 4 + h:15 * 4 + h + 1], None, ALU.subtract, None,
               [("band", h), "rbb"], [("band", h)])
        tt("dve", rbs[:, 0:4], rbb[:, 31 * 4:31 * 4 + 4], rbb[:, 15 * 4:15 * 4 + 4], ALU.subtract, ["rbb"], ["rbs"])
        act(ag[:], ag[:], AF.Copy, ["ag"], ["ag"], scale=ALPHA)
        act(ab[:], ab[:], AF.Copy, ["ab"], ["ab"], scale=ALPHA)
        dump("mod", mod_sb[0:2, :], [2, 6 * D], ["mod_sb"])
        dump("band", band[:, 0, :], [128, 1152], [("band", 0)])
        dump("lamc", lamc[:], [128, 8], ["lamc"])

    if lvl == 0:
        P.finish()
        return dbg_d

    win_v = win_d.rearrange("(k p) n -> p k n", p=128)
    wout_v = wout_d.rearrange("(k p) n -> p k n", p=128)

    for b in range(NB):
        with P.scope():
            bufA = P.sbuf("bufA", [128, 8, S], BF16)
            with P.scope():
                cqT = P.sbuf("cqT", [128, 2, S], BF16)
                ckvT = P.sbuf("ckvT", [128, S], BF16)
                ropeT = P.sbuf("ropeT", [128, 2, S], F32)
                krT = P.sbuf("krT", [128, S], BF16)
                dqT = P.sbuf("dqT", [128, 4, S], BF16)
                dkT = P.sbuf("dkT", [128, 4, S], BF16)
                dvx = P.sbuf("dvx", [128, NT, 512], BF16)

                R = slice(64, 96)
                with P.scope():
                    xs = P.sbuf("xs", [128, 4, D], F32)
                    wj = [P.sbuf("wj%d" % i, [128, 8, 512], BF16) for i in range(2)]
                    sq = [P.sbuf("sq%d" % i, [128, 512], F32) for i in range(2)]
                    rs = [P.sbuf("rs%d" % i, [128, 512], F32) for i in range(2)]
                    t1 = [P.sbuf("t1_%d" % i, [128, 512], F32) for i in range(2)]
                    ang, angc, rtmp = sq[0], sq[1], rs[0]
                    kang, kangc, ktmp, kpi = ("sq", 0), ("sq", 1), ("rs", 0), ("rs", 1)
                    pi_ = rs[1][:].bitcast(I32)
                    for q in range(4):
                        dma("act", pi_[q * 32:(q + 1) * 32, :], pos_d[b:b + 1, q * 512:(q + 1) * 512].partition_broadcast(32),
                            [], [kpi], "posi")
                    cp("dve", ang[:], pi_[:, :], [kpi], [kang])
                    ts("dve", ang[:], ang[:], colc[:, 0:1], None, ALU.mult, None, [kang, "colc"], [kang])
                    ts("dve", angc[:], ang[:], math.pi / 2.0, None, ALU.add, None, [kang], [kangc])
                    for which, (tab, tkey) in enumerate(((ang, kang), (angc, kangc))):
                        ts("dve", rtmp[:], tab[:], 1.0 / (2.0 * math.pi), None, ALU.mult, None, [tkey], [ktmp])
                        cp("dve", pi_[:, :], rtmp[:], [ktmp], [kpi])
                        cp("dve", rtmp[:], pi_[:, :], [kpi], [ktmp])
                        stt(tab[:], rtmp[:], -TWO_PI_HI, tab[:], ALU.mult, ALU.add, [ktmp, tkey], [tkey])
                        stt(tab[:], rtmp[:], -TWO_PI_LO, tab[:], ALU.mult, ALU.add, [ktmp, tkey], [tkey])
                        ts("dve", rtmp[:], tab[:], math.pi, -2.0 * math.pi, ALU.is_gt, ALU.mult, [tkey], [ktmp])
                        tt("dve", tab[:], tab[:], rtmp[:], ALU.add, [tkey, ktmp], [tkey])
                        ts("dve", rtmp[:], tab[:], -math.pi, 2.0 * math.pi, ALU.is_lt, ALU.mult, [tkey], [ktmp])
                        tt("dve", tab[:], tab[:], rtmp[:], ALU.add, [tkey, ktmp], [tkey])
                        if which == 0:
                            act(tab[:], tab[:], AF.Sin, [tkey, "colc"], [tkey], scale=colc[:, 1:2])
                        else:
                            act(tab[:], tab[:], AF.Sin, [tkey], [tkey])
                        for q in range(4):
                            dma("act", ropeT[R, 1 - which, q * 512:(q + 1) * 512], tab[q * 32:(q + 1) * 32, :],
                                [tkey], [("rope", 1 - which)], "ropew")
                    for g in range(4):
                        dma("sp", xs[:], x_d[b, g * 512:(g + 1) * 512, :].rearrange("(t p) d -> p t d", p=128),
                            [], ["xs"], "xs")
                        for k in range(8):
                            pb = PSP.get("s")
                            for t4 in range(4):
                                tr(ps[pb][:, t4 * 128:(t4 + 1) * 128], xs[:, t4, k * 128:(k + 1) * 128], ident_f[:],
                                   ["xs", "ident_f"], [("ps", pb)])
                            o_ap = bufA[:, k, g * 512:(g + 1) * 512]
                            if k % 2 == 0:
                                act(o_ap, ps[pb][:], AF.Identity, [("ps", pb), "modcol"], [("uT", g)],
                                    bias=modcol[:, b, k:k + 1], scale=modcol[:, b, 8 + k:9 + k])
                            else:
                                ts("dve", o_ap, ps[pb][:], modcol[:, b, 8 + k:9 + k], modcol[:, b, k:k + 1],
                                   ALU.mult, ALU.add, [("ps", pb), "modcol"], [("uT", g)])
                    dump("uT%d" % b, bufA[:, 0, :], [128, S], [("uT", g) for g in range(4)], BF16)

                    def proj_mm(pb, wt, wkey, c0, m, g):
                        for k in range(8):
                            mm(ps[pb][0:m, :], wt[:, k, c0:c0 + m], bufA[:, k, g * 512:(g + 1) * 512], k == 0, k == 7,
                               [wkey, ("uT", g)], [("ps", pb)])

                    for chunk in range(4):
                        w = wj[chunk % 2]
                        wkey = "wj%d" % (chunk % 2)
                        dma("pool", w[:], win_v[:, :, chunk * 512:(chunk + 1) * 512], [], [wkey], wkey)
                        for g in range(4):
                            G = slice(g * 512, (g + 1) * 512)
                            if chunk == 0:
                                pq = [PSP.get("a"), PSP.get("a")]
                                for c in range(2):
                                    proj_mm(pq[c], w, wkey, c * 128, 128, g)
                                    act(sq[c][:], ps[pq[c]][:], AF.Square, [("ps", pq[c])], [("sq", c)])
                                pr = PSP.get("m")
                                for c in range(2):
                                    mm(ps[pr][:], ones_f[:], sq[c][:], c == 0, c == 1, ["ones_f", ("sq", c)], [("ps", pr)])
                                act_pow(rs[0][:], ps[pr][:], [("ps", pr), "epsc"], [("rs", 0)], -0.5, scale=1.0 / 256.0, bias=epsc[:, 0:1])
                                for c in range(2):
                                    tt("dve", cqT[:, c, G], ps[pq[c]][:], rs[0][:], ALU.mult, [("ps", pq[c]), ("rs", 0)],
                                       [("cqT", g)])
                                pk = PSP.get("a")
                                proj_mm(pk, w, wkey, 256, 128, g)
                                act(sq[0][:], ps[pk][:], AF.Square, [("ps", pk)], [("sq", 0)])
                                pr = PSP.get("m")
                                mm(ps[pr][:], ones_f[:], sq[0][:], True, True, ["ones_f", ("sq", 0)], [("ps", pr)])
                                act_pow(rs[1][:], ps[pr][:], [("ps", pr), "epsc"], [("rs", 1)], -0.5, scale=1.0 / 128.0, bias=epsc[:, 0:1])
                                tt("dve", ckvT[:, G], ps[pk][:], rs[1][:], ALU.mult, [("ps", pk), ("rs", 1)], [("ckvT", g)])
                                pa = PSP.get("a")
                                pb2 = PSP.get("a")
                                proj_mm(pa, w, wkey, 320, 96, g)
                                proj_mm(pb2, w, wkey, 416, 96, g)
                                tt("dve", t1[0][R, :], ps[pa][R, :], ropeT[R, 0, G], ALU.mult,
                                   [("ps", pa), ("rope", 0)], [("t1", 0)])
                                tt("dve", t1[1][R, :], ps[pb2][R, :], ropeT[R, 1, G], ALU.mult,
                                   [("ps", pb2), ("rope", 1)], [("t1", 1)])
                                tt("pool", krT[R, G], t1[0][R, :], t1[1][R, :], ALU.add, [("t1", 0), ("t1", 1)], [("krT", g)])
                            elif chunk in (1, 2):
                                dst = dqT if chunk == 1 else dkT
                                nm = "dqT" if chunk == 1 else "dkT"
                                for h in range(4):
                                    pq_ = PSP.get("a")
                                    proj_mm(pq_, w, wkey, h * 128, 128, g)
                                    if h % 2 == 0:
                                        cp("act", dst[:, h, G], ps[pq_][:], [("ps", pq_)], [(nm, h, g)])
                                    else:
                                        cp("dve", dst[:, h, G], ps[pq_][:], [("ps", pq_)], [(nm, h, g)])
                            else:
                                for t4 in range(4):
                                    t = g * 4 + t4
                                    pv = PSP.get("a")
                                    for k in range(8):
                                        mm(ps[pv][:], bufA[:, k, t * 128:(t + 1) * 128], w[:, k, :], k == 0, k == 7,
                                           [wkey, ("uT", g)], [("ps", pv)])
                                    if t4 % 2 == 0:
                                        cp("act", dvx[:, t, :], ps[pv][:], [("ps", pv)], [("dvx", t)])
                                    else:
                                        cp("dve", dvx[:, t, :], ps[pv][:], [("ps", pv)], [("dvx", t)])
                    dump("cqT%d" % b, cqT[:, 0, :], [128, S], [("cqT", g) for g in range(4)], BF16)
                    dump("ckvT%d" % b, ckvT[:, :], [128, S], [("ckvT", g) for g in range(4)], BF16)
                    dump("krT%d" % b, krT[:, :], [128, S], [("krT", g) for g in range(4)], BF16)
                    dump("dqT%d" % b, dqT[:, 1, :], [128, S], [("dqT", 1, g) for g in range(4)], BF16)
                    dump("dvx%d" % b, dvx[:, 3, :], [128, 512], [("dvx", 3)], BF16)
                    dump("rope%d" % b, ropeT[:, 0, :], [128, S], [("rope", 0)])

                if lvl == 1:
                    continue

                with P.scope():
                    kT = [P.sbuf("kT%d" % i, [128, S], BF16) for i in range(2)]
                    qT = [P.sbuf("qT%d" % i, [128, S], BF16) for i in range(2)]
                    vh = [P.sbuf("vh%d" % i, [128, NT, 128], BF16) for i in range(2)]
                    NPT = 6
                    pT = [P.sbuf("pT%d" % i, [128, 512], BF16) for i in range(NPT)]
                    tmpb = [P.sbuf("tmpb%d" % i, [128, 512], F32) for i in range(2)]
                    osb = [P.sbuf("osb%d" % i, [128, 512], F32) for i in range(2)]
                    pctr = [0]
                    tctr = [0]
                    gctr = [0]

                    bcs = [P.sbuf("bcs%d" % i, [128, 512], F32) for i in range(2)]
                    rT = [P.sbuf("rT%d" % i, [128, 4], F32) for i in range(2)]
                    voff_of = {}
                    memset("pool", vh[0][:, :, 64:65], 1.0, [("vh", 0)])
                    memset("pool", vh[1][:, :, 0:64], 0.0, [("vh", 1)])
                    memset("pool", vh[1][:, :, 0:1], 1.0, [("vh", 1)])

                    PSP.cls = {"s": [0, 1, 2, 7], "a": [3, 4], "m": [5, 6]}

                    def mla_prep_pieces(h):
                        par = h % 2
                        kTh, qTh, vhh = kT[par], qT[par], vh[par]
                        voff = 0 if par == 0 else 64
                        pieces = []

                        def k_piece(g):
                            G = slice(g * 512, (g + 1) * 512)
                            pk = PSP.get("m")
                            mm(ps[pk][0:64, :], wukv_b[:, h * 64:(h + 1) * 64], ckvT[:, G], True, True,
                               ["wukv_b", ("ckvT", g)], [("ps", pk)])
                            cp("dve", kTh[0:64, G], ps[pk][0:64, :], [("ps", pk)], [("kT", par)])

                        def kr_piece():
                            cp("dve", kTh[R, :], krT[R, :], [("krT", g) for g in range(4)], [("kT", par)])

                        def v_piece(half):
                            pv = PSP.get("m")
                            for t8 in range(8):
                                t = half * 8 + t8
                                mm(ps[pv][:, t8 * 64:(t8 + 1) * 64], ckvT[:, t * 128:(t + 1) * 128],
                                   wukv_b[:, 512 + h * 64:512 + (h + 1) * 64], True, True,
                                   ["wukv_b", ("ckvT", t // 4)], [("ps", pv)])
                            cp("dve", vhh[:, half * 8:(half + 1) * 8, voff:voff + 64],
                               ps[pv][:].rearrange("p (t d) -> p t d", d=64), [("ps", pv)], [("vh", par)])

                        def q_piece(g):
                            G = slice(g * 512, (g + 1) * 512)
                            pa = PSP.get("m")
                            pb2 = PSP.get("m")
                            for c in range(2):
                                mm(ps[pa][0:96, :], wuq_b[:, c, h * 192:h * 192 + 96], cqT[:, c, G], c == 0, c == 1,
                                   ["wuq_b", ("cqT", g)], [("ps", pa)])
                            for c in range(2):
                                mm(ps[pb2][0:96, :], wuq_b[:, c, h * 192 + 96:h * 192 + 192], cqT[:, c, G], c == 0, c == 1,
                                   ["wuq_b", ("cqT", g)], [("ps", pb2)])
                            cp("dve", qTh[0:64, G], ps[pa][0:64, :], [("ps", pa)], [("qT", par, g)])
                            tt("dve", tmpb[0][R, :], ps[pa][R, :], ropeT[R, 0, G], ALU.mult, [("ps", pa), ("rope", 0)], [("tmpb", 0)])
                            tt("dve", tmpb[1][R, :], ps[pb2][R, :], ropeT[R, 1, G], ALU.mult, [("ps", pb2), ("rope", 1)], [("tmpb", 1)])
                            tt("pool", qTh[R, G], tmpb[0][R, :], tmpb[1][R, :], ALU.add, [("tmpb", 0), ("tmpb", 1)], [("qT", par, g)])

                        for g in range(4):
                            pieces.append(lambda g=g: k_piece(g))
                        pieces.append(kr_piece)
                        for half in range(2):
                            pieces.append(lambda half=half: v_piece(half))
                        for g in range(4):
                            pieces.append(lambda g=g: q_piece(g))
                        return pieces

                    def mla_prep(h):
                        for pc in mla_prep_pieces(h):
                            pc()

                    prep_cache = {}

                    LOOK = 3

                    def run_pipeline(blocks, stage_A, stage_BC):
                        info = [dict() for _ in blocks]
                        deferred = []
                        nblk = len(blocks)
                        for step in range(nblk + LOOK):
                            if step < nblk:
                                stage_A(blocks, info, step)
                            j = step - LOOK
                            if j >= 0:
                                stage_BC(blocks, info, j, step, deferred)
                            while deferred and deferred[0][0] <= step:
                                deferred.pop(0)[1]()
                        while deferred:
                            deferred.pop(0)[1]()

                    def mla_A(blocks, info, i):
                        h, g, kt, st = blocks[i]
                        G = slice(g * 512, (g + 1) * 512)
                        pss = PSP.get("s")
                        info[i]["pss"] = pss
                        par = h % 2
                        key = g * NT + kt
                        if h + 1 < 8 and key >= LOOK and (key - LOOK) % 4 == 0:
                            if h + 1 not in prep_cache:
                                prep_cache[h + 1] = mla_prep_pieces(h + 1)
                            pi_ = (key - LOOK) // 4
                            if pi_ < len(prep_cache[h + 1]):
                                prep_cache[h + 1][pi_]()
                        if WARM_DUMMY:
                            mm(ps[pss][:, 0:WARM_DUMMY], ident_b[:, :], ident_b[:, 0:WARM_DUMMY], True, True,
                               ["ident_b"], [("ps", pss)])
                        mm(ps[pss][:], kT[par][0:96, kt * 128:(kt + 1) * 128], qT[par][0:96, G], True, True,
                           [("kT", par), ("qT", par, g)], [("ps", pss)])

                    def mla_BC(blocks, info, i, step, deferred):
                        h, g, kt, st = blocks[i]
                        G = slice(g * 512, (g + 1) * 512)
                        pss = info[i]["pss"]
                        pi = pctr[0] % NPT
                        pctr[0] += 1
                        par = h % 2
                        voff = 0 if par == 0 else 64
                        M = 65 if par == 0 else 128
                        act(pT[pi][:], ps[pss][:], AF.Exp, [("ps", pss)], [("pT", pi)], scale=MLA_SCALE)
                        if kt == 0:
                            st["po"] = PSP.get("a")
                        po = st["po"]
                        mm(ps[po][0:M, :], vh[par][:, kt, 0:M], pT[pi][:], kt == 0, kt == NT - 1,
                           [("vh", par), ("pT", pi)], [("ps", po)])
                        if kt == NT - 1:
                            srow = 64 if par == 0 else 0
                            ri = gctr[0] % 2
                            gctr[0] += 1
                            SR = slice(srow, srow + 1)
                            ER = slice(0, 65) if par == 0 else slice(0, 128)
                            cp("dve", osb[ri][ER, :], ps[po][ER, :], [("ps", po)], [("rrow", ri), ("osb", ri)])
                            slot = (gctr[0] - 1) % 8
                            dma("sp", rs_d[slot:slot + 1, :], osb[ri][SR, :], [("rrow", ri)], [("rs_d", slot)], "rsw%d" % ri)
                            dma("sp", rT[ri][:, :], rs_d[slot:slot + 1, :].rearrange("o (p f) -> (o p) f", f=4),
                                [("rs_d", slot)], [("rT", ri)], "rT%d" % ri)

                            def post1b(ri=ri, slot=slot):
                                recip(rT[ri][:, :], rT[ri][:, :], [("rT", ri)], [("rT", ri)])
                                dma("pool", rs2_d[slot:slot + 1, :].rearrange("o (p f) -> (o p) f", f=4), rT[ri][:, :],
                                    [("rT", ri)], [("rs2_d", slot)], "rs2w%d" % ri)
                                dma("pool", bcs[ri][voff_of[ri]:voff_of[ri] + 64, :],
                                    rs2_d[slot:slot + 1, :].partition_broadcast(64),
                                    [("rs2_d", slot)], [("bcs", ri)], "bcs%d" % ri)
                            voff_of[ri] = voff
                            deferred.append((step + 12, post1b))

                            def post2(ri=ri, voff=voff, h=h, G=G, g=g):
                                tt("dve", bufA[voff:voff + 64, h // 2, G], osb[ri][voff:voff + 64, :],
                                   bcs[ri][voff:voff + 64, :], ALU.mult, [("osb", ri), ("bcs", ri)], [("oT", g)])
                            deferred.append((step + 26, post2))
                            deferred.sort(key=lambda x: x[0])

                    mla_prep(0)
                    blocks = [(h, g, kt, st) for h in range(8) for g in range(4) for st in ({},) for kt in range(NT)]
                    run_pipeline(blocks, mla_A, mla_BC)

                with P.scope():
                    NPT = 6
                    pT = [P.sbuf("pTd%d" % i, [128, 512], BF16) for i in range(NPT)]
                    rin = [P.sbuf("rind%d" % i, [128, 512], F32) for i in range(2)]
                    dtm = [P.sbuf("dtmd%d" % i, [128, 512], F32) for i in range(6)]
                    gctr = [0]
                    dqp = [[P.sbuf("dqp%d_%d" % (i, m), [128, S], BF16) for m in range(2)] for i in range(2)]
                    pctr = [0]
                    tctr = [0]
                    for i in range(2):
                        memset("pool", dqp[i][0][64:128, :], 0.0, [("dqp", i)])
                        memset("pool", dqp[i][1][0:64, :], 0.0, [("dqp", i)])
                    PSP.cls = {"s": [0, 1, 2, 7], "a": [3, 4, 5, 6], "m": []}

                    def dif_prep(h):
                        par = h % 2
                        cp("dve", dqp[par][0][0:64, :], dqT[0:64, h, :], [("dqT", h, g) for g in range(4)], [("dqp", par)])
                        cp("dve", dqp[par][1][64:128, :], dqT[64:128, h, :], [("dqT", h, g) for g in range(4)], [("dqp", par)])

                    def dif_A(blocks, info, i):
                        h, g, m, kt, st = blocks[i]
                        G = slice(g * 512, (g + 1) * 512)
                        pss = PSP.get("s")
                        info[i]["pss"] = pss
                        par = h % 2
                        if g == 0 and m == 0 and kt == LOOK and h + 1 < 4:
                            dif_prep(h + 1)
                        mm(ps[pss][:], dkT[:, h, kt * 128:(kt + 1) * 128], dqp[par][m][:, G], True, True,
                           [("dkT", h, kt // 4), ("dqp", par)], [("ps", pss)])

                    def dif_BC(blocks, info, i, step, deferred):
                        h, g, m, kt, st = blocks[i]
                        G = slice(g * 512, (g + 1) * 512)
                        pss = info[i]["pss"]
                        pi = pctr[0] % NPT
                        pctr[0] += 1
                        delta = kt * 128 - g * 512
                        if -128 <= delta <= 512:
                            st0 = 512 - delta
                            stt(ps[pss][:], ps[pss][:], DIFF_SCALE, band[:, h, st0:st0 + 512], ALU.mult, ALU.add,
                                [("ps", pss), ("band", h)], [("ps", pss)])
                            act(pT[pi][:], ps[pss][:], AF.Exp, [("ps", pss)], [("pT", pi)])
                        else:
                            if delta > 0:
                                act(pT[pi][:], ps[pss][:], AF.Exp, [("ps", pss), "rbs"], [("pT", pi)],
                                    bias=rbs[:, h:h + 1], scale=DIFF_SCALE)
                            else:
                                act(pT[pi][:], ps[pss][:], AF.Exp, [("ps", pss)], [("pT", pi)], scale=DIFF_SCALE)
                        if m == 0 and kt == 0:
                            st["od"] = [PSP.get("a"), PSP.get("a")]
                            st["sb"] = [PSP.get("a"), PSP.get("a")]
                        od, sb = st["od"][m], st["sb"][m]
                        mm(ps[od][:], dvx[:, kt, h * 128:(h + 1) * 128], pT[pi][:], kt == 0, kt == NT - 1,
                           [("dvx", kt), ("pT", pi)], [("ps", od)])
                        mm(ps[sb][:], ones_b[:, :], pT[pi][:], kt == 0, kt == NT - 1,
                           ["ones_b", ("pT", pi)], [("ps", sb)])
                        if kt == NT - 1:
                            di = st.setdefault("di", gctr[0] % 2)
                            if m == 0:
                                gctr[0] += 1
                            dA, dB, dS = dtm[di * 3], dtm[di * 3 + 1], dtm[di * 3 + 2]
                            kA, kB, kS = ("dtm", di * 3), ("dtm", di * 3 + 1), ("dtm", di * 3 + 2)
                            def post1(m=m, od=od, sb=sb, h=h, G=G, g=g, dA=dA, dB=dB, dS=dS, kA=kA, kB=kB, kS=kS,
                                      step=step):
                                act_pow(rin[m][:], ps[sb][:], [("ps", sb)], [("rin", m)], -1.0)
                                if m == 0:
                                    tt("dve", dA[:], ps[od][:], rin[0][:], ALU.mult, [("ps", od), ("rin", 0)], [kA])
                                else:
                                    stt(dB[:], ps[od][:], lamc[:, 1:2], rin[1][:], ALU.mult, ALU.mult,
                                        [("ps", od), ("rin", 1), "lamc"], [kB])
                                    tt("dve", dA[:], dA[:], dB[:], ALU.add, [kA, kB], [kA])
                                    tt("dve", dS[:], dA[:], dA[:], ALU.mult, [kA], [kS])

                                    def post2d():
                                        pr = sb
                                        mm(ps[pr][:], ones_f[:], dS[:], True, True, ["ones_f", kS], [("ps", pr)])
                                        act_pow(dB[:], ps[pr][:], [("ps", pr), "epsc"], [kB], -0.5, scale=1.0 / 128.0,
                                                bias=epsc[:, 0:1])
                                        stt(bufA[:, 4 + h, G], dA[:], sublnc[:, 0:1], dB[:], ALU.mult, ALU.mult,
                                            [kA, kB, "sublnc"], [("oT", g)])
                                    deferred.append((step + 9, post2d))
                                    deferred.sort(key=lambda x: x[0])
                            deferred.append((step + 3, post1))
                            deferred.sort(key=lambda x: x[0])

                    dif_prep(0)
                    blocks = [(h, g, m, kt, st) for h in range(4) for g in range(4) for st in ({},)
                              for m in range(2) for kt in range(NT)]
                    run_pipeline(blocks, dif_A, dif_BC)
                    PSP.cls = {"s": [0, 1, 2], "a": [3, 4, 5], "m": [6, 7]}
                    for kk in (0, 1, 4, 7):
                        dump("oT%d_%d" % (kk, b), bufA[:, kk, :], [128, S], [("oT", g) for g in range(4)], BF16)

            if lvl == 2:
                continue

            with P.scope():
                wout_b = P.sbuf("wout_b", [128, 8, D], BF16)
                wst = [P.sbuf("wst%d" % i, [128, D], F32) for i in range(2)]
                gab = P.sbuf("gab", [128, D], F32)
                Gp = P.sbuf("Gp", [128, D], F32)
                Bp = P.sbuf("Bp", [128, D], F32)
                NBF = 3
                xs2 = [P.sbuf("xs2_%d" % i, [128, D], F32) for i in range(NBF)]
                pre = [P.sbuf("pre%d" % i, [128, D], F32) for i in range(NBF)]
                xn = [P.sbuf("xn%d" % i, [128, D], F32) for i in range(NBF)]
                acc = [P.sbuf("acc%d" % i, [128, D], F32) for i in range(NBF)]
                uf = [P.sbuf("uf%d" % i, [128, D], F32) for i in range(NBF)]
                ufb = [P.sbuf("ufb%d" % i, [128, D], BF16) for i in range(NBF)]
                ufT = [P.sbuf("ufT%d" % i, [128, 8, 128], F32) for i in range(NBF)]
                stats = [P.sbuf("stats%d" % i, [128, 20], F32) for i in range(NBF)]
                lg = [P.sbuf("lg%d" % i, [128, 32], F32) for i in range(NBF)]

                dma("sp", gab[:], mod_d[b:b + 1, 2048:3072].partition_broadcast(128), ["mod_d"], ["gab"], "e0")
                dma("sp", Gp[:], mod_d[b:b + 1, 4096:5120].partition_broadcast(128), ["mod_d"], ["Gp"], "e0")
                dma("sp", Bp[:], mod_d[b:b + 1, 3072:4096].partition_broadcast(128), ["mod_d"], ["Bp"], "e0")
                tt("dve", xn[0][:], ab[:], Gp[:], ALU.mult, ["ab", "Gp"], [("xn", 0)])
                stt(Bp[:], xn[0][:], 1.0 / ALPHA, Bp[:], ALU.mult, ALU.add, [("xn", 0), "Bp"], ["Bp"])
                tt("dve", xn[1][:], ag[:], Gp[:], ALU.mult, ["ag", "Gp"], [("xn", 1)])
                ts("dve", Gp[:], xn[1][:], 1.0 / ALPHA, None, ALU.mult, None, [("xn", 1)], ["Gp"])
                for k in range(8):
                    dma("sp", wst[k % 2][:], wout_v[:, k, :], [], [("wst", k % 2)], "wst%d" % (k % 2))
                    tt("dve", wout_b[:, k, :], wst[k % 2][:], gab[:], ALU.mult,
                       [("wst", k % 2), "gab"], ["wout_b"])

                def epi_s1(t):
                    i3 = t % NBF
                    g = t // 4
                    T = slice(t * 128, (t + 1) * 128)
                    dma("sp", xs2[i3][:], x_d[b, T, :], [], [("xs2", i3)], "xs2_%d" % i3)
                    for half in range(2):
                        H = slice(half * 512, (half + 1) * 512)
                        pm = PSP.get("a")
                        for k in range(8):
                            mm(ps[pm][:], bufA[:, k, T], wout_b[:, k, H], k == 0, k == 7, [("oT", g), "wout_b"], [("ps", pm)])
                        stt(pre[i3][:, H], xs2[i3][:, H], ALPHA, ps[pm][:], ALU.mult, ALU.add,
                            [("xs2", i3), ("ps", pm)], [("pre", i3)])
                        P.op("dve", lambda e, i3=i3, half=half, H=H: e.bn_stats(out=stats[i3][:, half * 6:(half + 1) * 6],
                                                                              in_=pre[i3][:, H]),
                             [("pre", i3)], [("stats", i3)])
                    P.op("dve", lambda e, i3=i3: e.bn_aggr(out=stats[i3][:, 12:14], in_=stats[i3][:, 0:12]),
                         [("stats", i3)], [("stats", i3)])
                    act_pow(stats[i3][:, 15:16], stats[i3][:, 13:14], [("stats", i3), "epsc"], [("rstd", i3)], -0.5,
                            bias=epsc[:, 1:2])

                def epi_s2(t):
                    i3 = t % NBF
                    ts("dve", stats[i3][:, 16:17], stats[i3][:, 12:13], stats[i3][:, 15:16], -1.0, ALU.mult, ALU.mult,
                       [("stats", i3), ("rstd", i3)], [("nmr", i3)])
                    act(xn[i3][:], pre[i3][:], AF.Identity, [("pre", i3), ("rstd", i3), ("nmr", i3)], [("xn", i3)],
                        bias=stats[i3][:, 16:17], scale=stats[i3][:, 15:16])

                def epi_s3(t):
                    i3 = t % NBF
                    tt("dve", uf[i3][:], xn[i3][:], Gp[:], ALU.mult, [("xn", i3), "Gp"], [("uf", i3)])
                    tt("dve", uf[i3][:], uf[i3][:], Bp[:], ALU.add, [("uf", i3), "Bp"], [("uf", i3)])
                    tt("dve", acc[i3][:], xn[i3][:], ag[:], ALU.mult, [("xn", i3), "ag"], [("acc", i3)])
                    tt("dve", acc[i3][:], acc[i3][:], ab[:], ALU.add, [("acc", i3), "ab"], [("acc", i3)])
                    dma("pool", acc_d[b * S + t * 128:b * S + (t + 1) * 128, :], acc[i3][:], [("acc", i3)], [("acc_d", b)],
                        "accw%d" % i3)
                    cp("act", ufb[i3][:], uf[i3][:], [("uf", i3)], [("ufb", i3)])
                    dma("pool", uf_d[b * S + t * 128:b * S + (t + 1) * 128, :], ufb[i3][:], [("ufb", i3)], [("uf_d", b)],
                        "ufw%d" % i3)

                def epi_s4(t):
                    i3 = t % NBF
                    for half in range(2):
                        pt_ = PSP.get("s")
                        for k4 in range(4):
                            k = half * 4 + k4
                            tr(ps[pt_][:, k4 * 128:(k4 + 1) * 128], uf[i3][:, k * 128:(k + 1) * 128], ident_f[:],
                               [("uf", i3), "ident_f"], [("ps", pt_)])
                        cp("act", ufT[i3][:, half * 4:(half + 1) * 4, :], ps[pt_][:].rearrange("p (k t) -> p k t", t=128),
                           [("ps", pt_)], [("ufT", i3)])

                def epi_s5(t):
                    i3 = t % NBF
                    pl = PSP.get("m")
                    for k in range(8):
                        mm(ps[pl][:, 0:NE], ufT[i3][:, k, :], wr_sb[:, k, :], k == 0, k == 7, [("ufT", i3), "wr_sb"],
                           [("ps", pl)])
                    P.op("dve", lambda e, i3=i3, pl=pl: e.reduce_max(out=lg[i3][:, 16:17], in_=ps[pl][:, 0:NE], axis=AX.X),
                         [("ps", pl)], [("lgm", i3)])
                    ts("dve", lg[i3][:, 17:18], lg[i3][:, 16:17], -1.0, None, ALU.mult, None, [("lgm", i3)], [("lgm", i3)])
                    act(lg[i3][:, 0:NE], ps[pl][:, 0:NE], AF.Exp, [("ps", pl), ("lgm", i3)], [("lg", i3)],
                        bias=lg[i3][:, 17:18], accum_out=lg[i3][:, 18:19])

                def epi_s6(t):
                    i3 = t % NBF
                    recip(lg[i3][:, 19:20], lg[i3][:, 18:19], [("lg", i3)], [("lgr", i3)])
                    ts("dve", aff_all[:, t, b * 32:b * 32 + NE], lg[i3][:, 0:NE], lg[i3][:, 19:20], None, ALU.mult, None,
                       [("lg", i3), ("lgr", i3)], ["aff_all"])
                    if dbg and t == 3:
                        dump("pre3_%d" % b, pre[i3][:], [128, D], [("pre", i3)])
                        dump("uf3_%d" % b, uf[i3][:], [128, D], [("uf", i3)])

                stages = [epi_s1, epi_s2, epi_s3, epi_s4, epi_s5, epi_s6]
                for step in range(NT + len(stages) - 1):
                    for si, fn in enumerate(stages):
                        t = step - si
                        if 0 <= t < NT:
                            fn(t)

        if lvl == 3:
            continue


    if lvl <= 3:
        P.finish()
        return dbg_d

    with P.scope():
        NP = 48
        posT = P.sbuf("posT", [128, 16, NP], F32)
        maskT = P.sbuf("maskT", [128, 16, NP], F32)
        tvb = P.sbuf("tvb", [128, NB, 16, 128], BF16)
        offc = P.sbuf("offc", [128, 4], F32)
        NW = 5
        PF = NW - 2
        wbuf = [P.sbuf("wbuf%d" % i, [128, 8, 1024], BF16) for i in range(NW)]

        loads = []
        for e_ in range(NE):
            for q_ in range(2):
                loads.append(("g", e_, q_))
                loads.append(("u", e_, q_))
            for q_ in range(2):
                loads.append(("d", e_, q_))
        issued = [0]

        def issue_upto(k):
            while issued[0] <= min(k, len(loads) - 1):
                j = issued[0]
                kind, e_, q_ = loads[j]
                i = j % NW
                if kind == "g":
                    src = wg_d[e_].rearrange("(k p) f -> p k f", p=128)[:, :, q_ * 1024:(q_ + 1) * 1024]
                elif kind == "u":
                    src = wu_d[e_].rearrange("(k p) f -> p k f", p=128)[:, :, q_ * 1024:(q_ + 1) * 1024]
                else:
                    src = wd_d[e_].rearrange("(k p) d -> p k d", p=128)[:, q_ * 8:(q_ + 1) * 8, :]
                dma("pool", wbuf[i][:], src, [], [("wbuf", i)], "wbuf%d" % i)
                issued[0] += 1

        def use(j0, n):
            issue_upto(j0 + PF)
            return [(j0 + t) % NW for t in range(n)]


        issue_upto(PF)
        with P.scope():
            affT = P.sbuf("affT", [NP, S], F32)
            work = P.sbuf("work", [NP, S], F32)
            maskE = P.sbuf("maskE", [NP, S], F32)
            posE = P.sbuf("posE", [NP, S], F32)
            onesE = P.sbuf("onesE", [NP, S], F32)
            m8 = P.sbuf("m8", [NP, 8], F32)
            r1 = P.sbuf("r1", [128, 16, NE], F32)
            gb = P.sbuf("gb", [128, 16, NE], BF16)
            for g in range(4):
                pa_ = PSP.get("m")
                for t4 in range(4):
                    t = g * 4 + t4
                    tr(ps[pa_][0:NP, t4 * 128:(t4 + 1) * 128], aff_all[:, t, :], ident_f[:], ["aff_all", "ident_f"],
                       [("ps", pa_)])
                cp("act", affT[:, g * 512:(g + 1) * 512], ps[pa_][0:NP, :], [("ps", pa_)], ["affT"])
            cp("pool", work[:], affT[:], ["affT"], ["work"])
            memset("pool", onesE[:], 1.0, ["onesE"])
            for r in range(CAP // 8):
                P.op("dve", lambda e: e.max(out=m8[:], in_=work[:]), ["work"], ["m8"])
                if r < CAP // 8 - 1:
                    P.op("dve", lambda e: e.match_replace(out=work[:], in_to_replace=m8[:], in_values=work[:], imm_value=-1.0),
                         ["work", "m8"], ["work"])
            ts("dve", maskE[:], affT[:], m8[:, 7:8], None, ALU.is_ge, None, ["affT", "m8"], ["maskE"])
            P.op("dve", lambda e: e.tensor_tensor_scan(out=posE[:], data0=onesE[:], data1=maskE[:], initial=-1.0,
                                                       op0=ALU.mult, op1=ALU.add), ["onesE", "maskE"], ["posE"])
            for src, dstT, nm in ((posE, posT, "posT"), (maskE, maskT, "maskT")):
                skey = "posE" if nm == "posT" else "maskE"
                for g4 in range(4):
                    pa_ = PSP.get("m")
                    for t4 in range(4):
                        t = g4 * 4 + t4
                        tr(ps[pa_][:, t4 * NP:(t4 + 1) * NP], src[0:NP, t * 128:(t + 1) * 128], ident_f[0:NP, 0:NP],
                           [skey, "ident_f"], [("ps", pa_)])
                    cp("act", dstT[:, g4 * 4:(g4 + 1) * 4, :], ps[pa_][:, 0:4 * NP].rearrange("p (t e) -> p t e", e=NP),
                       [("ps", pa_)], [nm])
            memset("pool", tvb[:], 0.0, ["tvb"])
            for bb in range(NB):
                cp("pool", tvb[:, bb, :, 0], tok_iota[:, 0:16], ["tok_iota"], ["tvb"])
                cp("pool", tvb[:, bb, :, 1], tok_iota[:, 16:32], ["tok_iota"], ["tvb"])
                av = aff_all[:, :, bb * 32:bb * 32 + NE]
                gview = tvb[:, bb, :, 2:2 + 3 * NE].rearrange("p t (e j) -> p t e j", j=3)
                cp("dve", gb[:], av, ["aff_all"], ["gb"])
                cp("dve", gview[:, :, :, 0], gb[:], ["gb"], ["tvb"])
                tt("dve", r1[:], av, gb[:], ALU.subtract, ["aff_all", "gb"], ["r1"])
                cp("dve", gb[:], r1[:], ["r1"], ["gb"])
                cp("dve", gview[:, :, :, 1], gb[:], ["gb"], ["tvb"])
                tt("dve", r1[:], r1[:], gb[:], ALU.subtract, ["r1", "gb"], ["r1"])
                cp("dve", gview[:, :, :, 2], r1[:], ["r1"], ["tvb"])
            memset("pool", offc[:, 0:2], 0.0, ["offc"])
            memset("pool", offc[:, 2:4], float(S), ["offc"])
            dump("posT", posT[:].rearrange("p t e -> p (t e)"), [128, 16 * NP], ["posT"])

        NSEL = 4
        sel = [P.sbuf("sel%d" % i, [128, 256], BF16) for i in range(NSEL)]
        Rsb = [P.sbuf("Rsb%d" % i, [128, 512], F32) for i in range(1)]
        idxf4 = [P.sbuf("idxf4_%d" % i, [128, 8], F32) for i in range(2)]
        vsb = [P.sbuf("vsb%d" % i, [128, 4, 8], F32) for i in range(2)]
        selctr = [0]

        class RouteTask:
            def __init__(self, e_):
                self.e = e_
                self.pend = []
                self.i = 0
                self.bank = None

            def step(self):
                e_ = self.e
                if self.i > 16:
                    return
                if self.i == 0:
                    self.bank = PSP.get("m")
                bank = self.bank
                for (bb, t, si) in self.pend:
                    mm(ps[bank][:, bb * 256:(bb + 1) * 256], tvb[:, bb, t, :], sel[si][:], t == 0, t == NT - 1,
                       ["tvb", ("sel", si)], [("ps", bank)])
                self.pend = []
                if self.i < 16:
                    for j in range(2):
                        q = self.i * 2 + j
                        bb, t = q // 16, q % 16
                        si = selctr[0] % NSEL
                        selctr[0] += 1
                        col = bb * 32 + e_
                        ts("dve", sel[si][:], slot_iota[:], posT[:, t, col:col + 1], maskT[:, t, col:col + 1],
                           ALU.is_equal, ALU.mult, ["slot_iota", "posT", "maskT"], [("sel", si)])
                        self.pend.append((bb, t, si))
                else:
                    ri = e_ % 2
                    cp("act", Rsb[0][:], ps[bank][:, :], [("ps", bank)], [("Rsb", 0)])
                    pb_ = PSP.get("m")
                    for j in range(4):
                        tr(ps[pb_][:, j * 128:(j + 1) * 128], Rsb[0][:, j * 128:(j + 1) * 128], ident_f[:],
                           [("Rsb", 0), "ident_f"], [("ps", pb_)])
                    v = ps[pb_][:, :].rearrange("p (j c) -> p j c", c=128)
                    f4 = idxf4[ri]
                    g0 = 2 + 3 * e_
                    vs = vsb[ri]
                    cp("act", vs[:, :, 0:2], v[:, :, 0:2], [("ps", pb_)], [("vs", ri)])
                    cp("act", vs[:, :, 2:5], v[:, :, g0:g0 + 3], [("ps", pb_)], [("vs", ri)])
                    stt(f4[:, 0:4], vs[:, :, 1], 128.0, vs[:, :, 0], ALU.mult, ALU.add, [("vs", ri)], [("f4", ri)])
                    tt("dve", f4[:, 0:4], f4[:, 0:4], offc[:, 0:4], ALU.add, [("f4", ri), "offc"], [("f4", ri)])
                    tt("dve", f4[:, 4:8], vs[:, :, 2], vs[:, :, 3], ALU.add, [("vs", ri)], [("f4g", ri)])
                    gv = g_sb[:, :].rearrange("p (b e c) -> p b e c", b=2, e=NE)[:, :, e_, :]
                    iv = idx_i[:, :].rearrange("p (b e c) -> p b e c", b=2, e=NE)[:, :, e_, :]
                    tt("dve", gv, f4[:, 4:8].rearrange("p (b c) -> p b c", c=2),
                       vs[:, :, 4].rearrange("p (b c) -> p b c", c=2),
                       ALU.add, [("f4g", ri), ("vs", ri)], [("g_sb", e_)])
                    cp("dve", iv, f4[:, 0:4].rearrange("p (b c) -> p b c", c=2), [("f4", ri)], [("idx_i", e_)])
                self.i += 1

            def run_all(self):
                while self.i <= 16:
                    self.step()

        rtasks = [RouteTask(e_) for e_ in range(NE)]
        rtasks[0].run_all()
        rtasks[1].run_all()
        if dbg:
            for e_ in range(2, NE):
                rtasks[e_].run_all()
            dump("idxi", idx_i[:], [128, 64], [("idx_i", e_) for e_ in range(NE)], I32)
            dump("gsb", g_sb[:], [128, 64], [("g_sb", e_) for e_ in range(NE)])
        if lvl == 4:
            P.finish()
            return dbg_d

        xsg = [P.sbuf("xsg%d" % i, [128, D], BF16) for i in range(8)]
        xsT = [P.sbuf("xsT%d" % i, [128, 8, 512], BF16) for i in range(2)]
        hT = P.sbuf("hT", [128, 16, 512], BF16)
        sgt = [P.sbuf("sgt%d" % i, [128, 512], F32) for i in range(2)]
        ysb = [P.sbuf("ysb%d" % i, [128, D], F32) for i in range(2)]

        def gather(e_):
            xi = e_ % 2
            for sc in range(4):
                bb, c = sc // 2, sc % 2
                col = bb * 32 + e_ * 2 + c
                gi = xi * 4 + sc
                P.op("pool", lambda e, gi=gi, col=col: e.indirect_dma_start(
                    out=xsg[gi][:], out_offset=None, in_=uf_d[:, :],
                    in_offset=bass.IndirectOffsetOnAxis(ap=idx_i[:, col:col + 1], axis=0)),
                    [("idx_i", e_), ("uf_d", 0), ("uf_d", 1)], [("xsg", gi)], dma="xsg%d" % gi)

        issue_upto(PF)
        gather(0)
        lctr = 0
        yctr = 0
        prev_scat = {0: [], 1: []}
        for e_ in range(NE):
            xi = e_ % 2
            for sc in range(4):
                gi = xi * 4 + sc
                pt_ = PSP.get("m")
                pview = ps[pt_].bitcast(BF16)
                for k in range(8):
                    tr(pview[:, k * 128:(k + 1) * 128], xsg[gi][:, k * 128:(k + 1) * 128], ident_b[:],
                       [("xsg", gi), "ident_b"], [("ps", pt_)])
                cp("act" if sc % 2 == 0 else "dve", xsT[xi][:, :, sc * 128:(sc + 1) * 128],
                   pview[:, 0:1024].rearrange("p (k t) -> p k t", t=128), [("ps", pt_)], [("xsT", xi)])
            if e_ + 1 < NE:
                gather(e_ + 1)
            for q_ in range(2):
                wgi, wui = use(lctr, 2)
                lctr += 2
                for f8 in range(8):
                    f = q_ * 8 + f8
                    if e_ + 2 < NE:
                        rtasks[e_ + 2].step()
                    pg = PSP.get("s")
                    pu = PSP.get("a")
                    for k in range(8):
                        mm(ps[pg][:], wbuf[wgi][:, k, f8 * 128:(f8 + 1) * 128], xsT[xi][:, k, :], k == 0, k == 7,
                           [("wbuf", wgi), ("xsT", xi)], [("ps", pg)])
                    for k in range(8):
                        mm(ps[pu][:], wbuf[wui][:, k, f8 * 128:(f8 + 1) * 128], xsT[xi][:, k, :], k == 0, k == 7,
                           [("wbuf", wui), ("xsT", xi)], [("ps", pu)])
                    si = f % 2
                    act(sgt[si][:], ps[pg][:], AF.Silu, [("ps", pg)], [("sgt", si)])
                    tt("dve", hT[:, f, :], sgt[si][:], ps[pu][:], ALU.mult, [("sgt", si), ("ps", pu)], [("hT", f)])
            if e_ + 2 < NE:
                rtasks[e_ + 2].run_all()
            wdi = use(lctr, 2)
            lctr += 2
            new_scat = {0: [], 1: []}
            for sc in range(4):
                bb, c = sc // 2, sc % 2
                col = bb * 32 + e_ * 2 + c
                yi = yctr % 2
                yctr += 1
                for half in range(2):
                    py = PSP.get("m")
                    for f in range(16):
                        mm(ps[py][:], hT[:, f, sc * 128:(sc + 1) * 128],
                           wbuf[wdi[f // 8]][:, f % 8, half * 512:(half + 1) * 512],
                           f == 0, f == 15, [("hT", f), ("wbuf", wdi[f // 8])], [("ps", py)])
                    stt(ysb[yi][:, half * 512:(half + 1) * 512], ps[py][:], g_sb[:, col:col + 1],
                        gfb[:, bb, half * 512:(half + 1) * 512], ALU.mult, ALU.mult,
                        [("ps", py), ("g_sb", e_), "gfb"], [("ysb", yi)])
                ref = P.op("pool", lambda e, yi=yi, col=col: e.indirect_dma_start(
                    out=acc_d[:, :], out_offset=bass.IndirectOffsetOnAxis(ap=idx_i[:, col:col + 1], axis=0),
                    in_=ysb[yi][:], in_offset=None, compute_op=ALU.add),
                    [("ysb", yi), ("idx_i", e_)], [], dma="scat%d" % yi, after=prev_scat[bb])
                new_scat[bb].append(ref)
            prev_scat = new_scat

    if lvl == 5:
        P.finish()
        return dbg_d

    with P.scope():
        g2 = P.sbuf("g2", [128, D], F32)
        b2 = P.sbuf("b2", [128, D], F32)
        dma("sp", g2[:], ln2_d[0:1, :].partition_broadcast(128), [], ["g2"], "f0")
        dma("sp", b2[:], ln2_d[1:2, :].partition_broadcast(128), [], ["b2"], "f0")
        NF = 6
        fin = [P.sbuf("fin%d" % i, [128, D], F32) for i in range(NF)]
        fo = [P.sbuf("fo%d" % i, [128, D], F32) for i in range(NF)]
        st2 = [P.sbuf("st2_%d" % i, [128, 20], F32) for i in range(NF)]

        def fin_s1(bt):
            b, t = bt // NT, bt % NT
            i3 = bt % NF
            dma("sp", fin[i3][:], acc_d[b * S + t * 128:b * S + (t + 1) * 128, :], [("acc_d", b)], [("fin", i3)],
                "fin%d" % i3)
            for half in range(2):
                P.op("dve", lambda e, i3=i3, half=half: e.bn_stats(out=st2[i3][:, half * 6:(half + 1) * 6],
                                                                   in_=fin[i3][:, half * 512:(half + 1) * 512]),
                     [("fin", i3)], [("st2", i3)])
            P.op("dve", lambda e, i3=i3: e.bn_aggr(out=st2[i3][:, 12:14], in_=st2[i3][:, 0:12]), [("st2", i3)], [("st2", i3)])
            act_pow(st2[i3][:, 15:16], st2[i3][:, 13:14], [("st2", i3), "epsc"], [("rstd2", i3)], -0.5, bias=epsc[:, 1:2])

        def fin_s2(bt):
            i3 = bt % NF
            ts("dve", st2[i3][:, 16:17], st2[i3][:, 12:13], st2[i3][:, 15:16], -1.0, ALU.mult, ALU.mult,
               [("st2", i3), ("rstd2", i3)], [("nmr2", i3)])
            act(fo[i3][:], fin[i3][:], AF.Identity, [("fin", i3), ("rstd2", i3), ("nmr2", i3)], [("fo", i3)],
                bias=st2[i3][:, 16:17], scale=st2[i3][:, 15:16])

        def fin_s3(bt):
            b, t = bt // NT, bt % NT
            i3 = bt % NF
            tt("dve", fo[i3][:], fo[i3][:], g2[:], ALU.mult, [("fo", i3), "g2"], [("fo", i3)])
            tt("dve", fo[i3][:], fo[i3][:], b2[:], ALU.add, [("fo", i3), "b2"], [("fo", i3)])
            dma("pool", out_d[b, t * 128:(t + 1) * 128, :], fo[i3][:], [("fo", i3)], ["out_d"], "outw%d" % i3)

        NTT = NB * NT
        for step in range(NTT + 2):
            if step < NTT:
                fin_s1(step)
            if 0 <= step - 1 < NTT:
                fin_s2(step - 1)
            if 0 <= step - 2 < NTT:
                fin_s3(step - 2)
    P.finish()
    return dbg_d


def _host_inputs(inp):
    f = lambda k: np.ascontiguousarray(np.asarray(inp[k], dtype=np.float32))
    w_in = f("w_in")[0]
    kr = w_in[:, 384:416]
    kr_sw = np.concatenate([kr[:, 16:32], kr[:, 0:16]], axis=1)
    w_in_ext = np.concatenate([w_in[:, 0:416], w_in[:, 320:384], kr_sw, w_in[:, 416:1952]], axis=1)
    assert w_in_ext.shape == (1024, 2048)
    w_uq = f("w_uq")[0].reshape(256, 8, 96)
    nope, rp = w_uq[:, :, 0:64], w_uq[:, :, 64:96]
    rp_sw = np.concatenate([rp[:, :, 16:32], rp[:, :, 0:16]], axis=2)
    w_uq_ext = np.concatenate([nope, rp, nope, rp_sw], axis=2).reshape(256, 1536)
    w_ukv = f("w_ukv")[0].reshape(128, 8, 128)
    w_ukv_r = np.concatenate([w_ukv[:, :, 0:64].reshape(128, 512), w_ukv[:, :, 64:128].reshape(128, 512)], axis=1)
    qn_col = f("mla_q_norm")[0].reshape(2, 128).T
    kvn_col = f("mla_kv_norm")[0].reshape(128, 1)
    lam_in = np.concatenate([f("diff_lq1")[0], f("diff_lk1")[0], f("diff_lq2")[0], f("diff_lk2")[0]]).reshape(1, 256)
    subln_col = f("diff_subln")[0].reshape(128, 1)
    ln1 = np.stack([f("ln1_g")[0], f("ln1_b")[0]])
    ln2 = np.stack([f("ln2_g")[0], f("ln2_b")[0]])
    ident = np.eye(128, dtype=np.float32)
    kl = np.arange(128, dtype=np.float32)[:, None]
    rel_iota = (kl - np.arange(384, dtype=np.float32)[None, :] + 128.0).astype(np.float32)
    freqs = (np.float32(10000.0) ** (-np.arange(16, dtype=np.float32) / np.float32(16))).astype(np.float32)
    cols = np.zeros((128, 4), np.float32)
    for p in range(128):
        cols[p, 0] = freqs[p % 16]
        cols[p, 1] = -1.0 if (p % 32) < 16 else 1.0
    slot_iota = np.tile(np.arange(256, dtype=np.float32)[None, :], (128, 1))
    tok_iota = np.concatenate([np.tile(np.arange(128, dtype=np.float32)[:, None], (1, 16)),
                               np.tile(np.arange(16, dtype=np.float32)[None, :], (128, 1))], axis=1).astype(np.float32)
    shared = {
        "rel_bias": f("rel_bias").reshape(1, 128),
        "w_ada": f("w_ada")[0], "b_ada": f("b_ada").reshape(1, 6144),
        "w_in_ext": np.ascontiguousarray(w_in_ext), "w_uq_ext": np.ascontiguousarray(w_uq_ext),
        "w_ukv_r": np.ascontiguousarray(w_ukv_r), "qn_col": np.ascontiguousarray(qn_col), "kvn_col": kvn_col,
        "lam_in": lam_in, "subln_col": subln_col, "w_out": f("w_out")[0], "ln1": ln1,
        "w_router": f("w_router")[0], "w_gate": f("w_gate")[0], "w_up": f("w_up")[0], "w_down": f("w_down")[0],
        "ln2": ln2, "ident": ident, "rel_iota": rel_iota, "cols": cols, "slot_iota": slot_iota, "tok_iota": tok_iota,
    }
    x = f("x")
    c = f("c")
    pos = np.ascontiguousarray(np.asarray(inp["positions"], dtype=np.int32))
    maps = []
    for core in range(8):
        m = dict(shared)
        m["x"] = np.ascontiguousarray(x[core * NB:(core + 1) * NB])
        cc = c[core * NB:(core + 1) * NB]
        m["cT"] = np.ascontiguousarray(cc.reshape(NB, 8, 128).transpose(2, 1, 0).reshape(128, 16))
        m["pos"] = np.ascontiguousarray(pos[core * NB:(core + 1) * NB])
        maps.append(m)
    return maps


def _build_nc(stage="full", dbg=False):
    P1 = Prog(None)
    build(P1, stage, dbg)
    nc = bass.Bass("TRN2", target_bir_lowering=False)
    P2 = Prog(nc, sig=P1.need)
    with P2.stack[0]:
        dbg_d = build(P2, stage, dbg)
    return nc, P2, dbg_d


def kernel(**inputs):
    stage = os.environ.get("KSTAGE", "full")
    dbg = os.environ.get("KDEBUG", "0") == "1"
    ncores = int(os.environ.get("KCORES", "8"))
    nc, P2, dbg_d = _build_nc(stage, dbg)
    maps = _host_inputs(inputs)[:ncores]
    res = run_bass_kernel_spmd(nc, maps, core_ids=list(range(ncores)))
    if dbg:
        kernel.last = res.results
    outs = [r["out"] for r in res.results]
    while len(outs) < 8:
        outs.append(np.zeros_like(outs[0]))
    return np.concatenate(outs, axis=0).astype(np.float32)
```
